# Optimizing a Trainium2 kernel written in Bass

```python
import jax
import jax.numpy as jnp
from jax import lax
import numpy as np

D_MODEL = 1024
BATCH = 2
SEQ = 8192
DEPTH = 2

RMS_EPS = 1e-6
MIX_W = D_MODEL
A_HEADS = 4
A_HEAD_K = 128
A_HEAD_V = MIX_W // 2 // A_HEADS
A_KW = A_HEADS * A_HEAD_K
A_W = A_HEADS * A_HEAD_V
HGRN_CHUNK = 64
B_GROUPS = 4
B_W = MIX_W // 2
B_GROUP_DIM = B_W // B_GROUPS
GMLP_CHUNK = 128
EVEN_IN = 2 * A_KW + 2 * A_W + 2 * B_W
C_HEADS = 4
Q_LORA = 256
KV_LORA = 128
C_NOPE = 128
C_ROPE = 64
C_QK = C_NOPE + C_ROPE
C_V = MIX_W // 2 // C_HEADS
C_W = C_HEADS * C_V
ROPE_THETA = 10000.0
ATTN_Q_BLOCK = 128
D_HEADS = 4
D_HEAD_DIM = MIX_W // 2 // D_HEADS
D_W = D_HEADS * D_HEAD_DIM
MOBA_BLOCK = 256
MOBA_TOPK = 3
MOBA_Q_CHUNK = 64
ODD_IN = Q_LORA + KV_LORA + C_ROPE + 3 * D_W
PEER_HEADS = 8
PEER_N_KEYS = 128
PEER_N_EXPERTS = PEER_N_KEYS * PEER_N_KEYS
PEER_TOPK = 16
PEER_HALF = 128
PEER_QDIM = 2 * PEER_HALF
PEER_CHUNK = 128

kernel_name = 'hybrid_hgrn2_gmlp_mla_moba_peer'


def rmsnorm(x, g):
    xf = x.astype(jnp.float32)
    y = xf * lax.rsqrt(jnp.mean(xf * xf, axis=-1, keepdims=True) + RMS_EPS)
    return y.astype(x.dtype) * g


def split_cols(z, sizes):
    cuts = [int(c) for c in np.cumsum(sizes)[:-1]]
    return jnp.split(z, cuts, axis=-1)


def rope(x, positions):
    half = x.shape[-1] // 2
    inv_freq = ROPE_THETA ** (-jnp.arange(half, dtype=jnp.float32) / half)
    ang = positions.astype(jnp.float32)[..., None] * inv_freq
    cos = jnp.cos(ang)[:, :, None, :]
    sin = jnp.sin(ang)[:, :, None, :]
    x1 = x[..., :half].astype(jnp.float32)
    x2 = x[..., half:].astype(jnp.float32)
    return jnp.concatenate([x1 * cos - x2 * sin, x2 * cos + x1 * sin], axis=-1).astype(x.dtype)


def hgrn_lower_bounds(lb_logits):
    p = jax.nn.softmax(lb_logits.astype(jnp.float32), axis=0)
    return jnp.cumsum(p, axis=0)[:DEPTH]


def hgrn2_recurrence(q, k, v, log_f):
    B, S, H, dk = q.shape
    dv = v.shape[-1]
    nc = S // HGRN_CHUNK

    def chunks(t):
        return t.reshape(B, nc, HGRN_CHUNK, H, t.shape[-1]).transpose(1, 0, 3, 2, 4)

    causal = jnp.tril(jnp.ones((HGRN_CHUNK, HGRN_CHUNK), dtype=bool))[:, :, None]

    def step(state, inp):
        qi, ki, vi, gi = inp
        b = jnp.cumsum(gi.astype(jnp.float32), axis=2)
        o_inter = jnp.einsum('bhtk,bhkv->bhtv', qi * jnp.exp(b), state)
        diff = b[:, :, :, None, :] - b[:, :, None, :, :]
        decay = jnp.exp(jnp.where(causal, diff, -jnp.inf))
        scores = jnp.einsum('bhtk,bhsk,bhtsk->bhts', qi, ki, decay)
        o = o_inter + jnp.einsum('bhts,bhsv->bhtv', scores, vi)
        b_last = b[:, :, -1:, :]
        k_dec = ki * jnp.exp(b_last - b)
        state = jnp.exp(b_last[:, :, 0, :])[..., None] * state + jnp.einsum('bhsk,bhsv->bhkv', k_dec, vi)
        return state, o

    state0 = jnp.zeros((B, H, dk, dv), jnp.float32)
    _, o = lax.scan(step, state0, (chunks(q), chunks(k), chunks(v), chunks(log_f)))
    return o.transpose(1, 0, 3, 2, 4).reshape(B, S, H, dv)


def causal_attention(q, k, v, scale):
    B, H, S, dqk = q.shape
    nq = S // ATTN_Q_BLOCK
    qb = q.reshape(B, H, nq, ATTN_Q_BLOCK, dqk).transpose(2, 0, 1, 3, 4)
    k_pos = jnp.arange(S)

    def block(args):
        i, qi = args
        s = jnp.einsum('bhqd,bhkd->bhqk', qi, k).astype(jnp.float32) * scale
        q_pos = i * ATTN_Q_BLOCK + jnp.arange(ATTN_Q_BLOCK)
        s = jnp.where(k_pos[None, :] <= q_pos[:, None], s, -jnp.inf)
        p = jax.nn.softmax(s, axis=-1).astype(v.dtype)
        return jnp.einsum('bhqk,bhkd->bhqd', p, v)

    o = lax.map(block, (jnp.arange(nq), qb))
    return o.transpose(1, 2, 0, 3, 4).reshape(B, H, S, v.shape[-1])


def moba_attention(q, k, v, scale):
    B, H, S, d = q.shape
    nb = -(-S // MOBA_BLOCK)
    sp = nb * MOBA_BLOCK
    pad = ((0, 0), (0, 0), (0, sp - S), (0, 0))
    q, k, v = jnp.pad(q, pad), jnp.pad(k, pad), jnp.pad(v, pad)
    kb = k.reshape(B, H, nb, MOBA_BLOCK, d)
    vb = v.reshape(B, H, nb, MOBA_BLOCK, d)
    k_mean = jnp.mean(kb.astype(jnp.float32), axis=3)
    gate = jnp.einsum('bhsd,bhnd->bhsn', q.astype(jnp.float32), k_mean)
    q_block = jnp.arange(sp) // MOBA_BLOCK
    past = jnp.arange(nb)[None, :] < q_block[:, None]
    gate = jnp.where(past, gate, -jnp.inf)
    topk = min(MOBA_TOPK, nb)
    _, sel = lax.top_k(gate, topk)
    valid = sel < q_block[:, None]
    nqc = sp // MOBA_Q_CHUNK

    def chunks(t):
        return t.reshape(B, H, nqc, MOBA_Q_CHUNK, t.shape[-1]).transpose(2, 0, 1, 3, 4)

    b_idx = jnp.arange(B)[:, None, None, None]
    h_idx = jnp.arange(H)[None, :, None, None]

    def chunk(args):
        i, qi, si, vi_ = args
        k_sel = kb[b_idx, h_idx, si]
        v_sel = vb[b_idx, h_idx, si]
        s_sel = jnp.einsum('bhqd,bhqnkd->bhqnk', qi, k_sel).astype(jnp.float32) * scale
        s_sel = jnp.where(vi_[..., None], s_sel, -jnp.inf).reshape(B, H, MOBA_Q_CHUNK, topk * MOBA_BLOCK)
        j = (i * MOBA_Q_CHUNK) // MOBA_BLOCK
        k_own = lax.dynamic_index_in_dim(kb, j, axis=2, keepdims=False)
        v_own = lax.dynamic_index_in_dim(vb, j, axis=2, keepdims=False)
        s_own = jnp.einsum('bhqd,bhkd->bhqk', qi, k_own).astype(jnp.float32) * scale
        q_pos = i * MOBA_Q_CHUNK + jnp.arange(MOBA_Q_CHUNK)
        k_pos = j * MOBA_BLOCK + jnp.arange(MOBA_BLOCK)
        s_own = jnp.where(k_pos[None, :] <= q_pos[:, None], s_own, -jnp.inf)
        p = jax.nn.softmax(jnp.concatenate([s_sel, s_own], axis=-1), axis=-1).astype(v.dtype)
        p_sel = p[..., :topk * MOBA_BLOCK].reshape(B, H, MOBA_Q_CHUNK, topk, MOBA_BLOCK)
        p_own = p[..., topk * MOBA_BLOCK:]
        return (jnp.einsum('bhqnk,bhqnkd->bhqd', p_sel, v_sel)
                + jnp.einsum('bhqk,bhkd->bhqd', p_own, v_own))

    o = lax.map(chunk, (jnp.arange(nqc), chunks(q), chunks(sel), chunks(valid)))
    return o.transpose(1, 2, 0, 3, 4).reshape(B, H, sp, d)[:, :, :S]


def hgrn_gmlp_mixer(x, lb, norm_mix, w_in, hgrn_out_norm, gmlp_v_norm, gmlp_w_s, gmlp_b_s, w_out):
    B, S, _ = x.shape
    xn = rmsnorm(x, norm_mix)
    q, fg, inp, og, u, v = split_cols(xn @ w_in, [A_KW, A_KW, A_W, A_W, B_W, B_W])
    f = lb + (1.0 - lb) * jax.nn.sigmoid(fg.astype(jnp.float32))
    o = hgrn2_recurrence(q.reshape(B, S, A_HEADS, A_HEAD_K),
                         (1.0 - f).reshape(B, S, A_HEADS, A_HEAD_K),
                         jax.nn.silu(inp).reshape(B, S, A_HEADS, A_HEAD_V),
                         jnp.log(f).reshape(B, S, A_HEADS, A_HEAD_K))
    o = rmsnorm(o.astype(x.dtype), hgrn_out_norm.reshape(A_HEADS, A_HEAD_V))
    out_a = o.reshape(B, S, A_W) * jax.nn.silu(og)
    u = jax.nn.gelu(u)
    v = rmsnorm(jax.nn.gelu(v).reshape(B, S, B_GROUPS, B_GROUP_DIM),
                gmlp_v_norm.reshape(B_GROUPS, B_GROUP_DIM))
    nc = S // GMLP_CHUNK
    w_causal = gmlp_w_s * jnp.tril(jnp.ones((GMLP_CHUNK, GMLP_CHUNK), gmlp_w_s.dtype))
    sv = jnp.einsum('gts,bnsgd->bntgd', w_causal, v.reshape(B, nc, GMLP_CHUNK, B_GROUPS, B_GROUP_DIM))
    sv = sv + gmlp_b_s.T[:, :, None]
    out_b = u * sv.reshape(B, S, B_W)
    return jnp.concatenate([out_a, out_b], axis=-1) @ w_out


def mla_moba_mixer(x, positions, norm_mix, w_in, cq_norm, ckv_norm, w_uq, w_ukv,
                   mla_q_norm, mla_k_norm, moba_q_norm, moba_k_norm, w_out):
    B, S, _ = x.shape
    xn = rmsnorm(x, norm_mix)
    c_q, c_kv, k_pe, q_d, k_d, v_d = split_cols(xn @ w_in, [Q_LORA, KV_LORA, C_ROPE, D_W, D_W, D_W])
    q_c = (rmsnorm(c_q, cq_norm) @ w_uq).reshape(B, S, C_HEADS, C_QK)
    kv_c = (rmsnorm(c_kv, ckv_norm) @ w_ukv).reshape(B, S, C_HEADS, C_NOPE + C_V)
    k_c = jnp.concatenate([kv_c[..., :C_NOPE],
                           jnp.broadcast_to(k_pe[:, :, None, :], (B, S, C_HEADS, C_ROPE))], axis=-1)
    v_c = kv_c[..., C_NOPE:]
    q_c = rmsnorm(q_c, mla_q_norm)
    k_c = rmsnorm(k_c, mla_k_norm)
    q_c = jnp.concatenate([q_c[..., :C_NOPE], rope(q_c[..., C_NOPE:], positions)], axis=-1)
    k_c = jnp.concatenate([k_c[..., :C_NOPE], rope(k_c[..., C_NOPE:], positions)], axis=-1)
    o_c = causal_attention(q_c.transpose(0, 2, 1, 3), k_c.transpose(0, 2, 1, 3),
                           v_c.transpose(0, 2, 1, 3), C_QK ** -0.5)
    out_c = o_c.transpose(0, 2, 1, 3).reshape(B, S, C_W)
    q_d = rmsnorm(q_d.reshape(B, S, D_HEADS, D_HEAD_DIM), moba_q_norm)
    k_d = rmsnorm(k_d.reshape(B, S, D_HEADS, D_HEAD_DIM), moba_k_norm)
    v_d = v_d.reshape(B, S, D_HEADS, D_HEAD_DIM)
    o_d = moba_attention(q_d.transpose(0, 2, 1, 3), k_d.transpose(0, 2, 1, 3),
                         v_d.transpose(0, 2, 1, 3), D_HEAD_DIM ** -0.5)
    out_d = o_d.transpose(0, 2, 1, 3).reshape(B, S, D_W)
    return jnp.concatenate([out_c, out_d], axis=-1) @ w_out


def peer_ffn(x, norm_ffn, w_query, sub_keys, expert_down, expert_up):
    B, S, D = x.shape
    xn = rmsnorm(x, norm_ffn)
    nt = S // PEER_CHUNK
    xc = xn.reshape(B, nt, PEER_CHUNK, D).transpose(1, 0, 2, 3)

    def chunk(xi):
        q = (xi @ w_query).reshape(B, PEER_CHUNK, PEER_HEADS, 2, PEER_HALF)
        s = jnp.einsum('bthpd,hpnd->bthpn', q, sub_keys)
        s1, i1 = lax.top_k(s[..., 0, :], PEER_TOPK)
        s2, i2 = lax.top_k(s[..., 1, :], PEER_TOPK)
        cand = (s1[..., :, None] + s2[..., None, :]).reshape(B, PEER_CHUNK, PEER_HEADS, PEER_TOPK * PEER_TOPK)
        top_s, top_c = lax.top_k(cand, PEER_TOPK)
        idx = (jnp.take_along_axis(i1, top_c // PEER_TOPK, axis=-1) * PEER_N_KEYS
               + jnp.take_along_axis(i2, top_c % PEER_TOPK, axis=-1))
        g = jax.nn.softmax(top_s.astype(jnp.float32), axis=-1)
        u = jnp.take(expert_down, idx, axis=0)
        act = jax.nn.gelu(jnp.einsum('btd,bthkd->bthk', xi, u).astype(jnp.float32))
        v_sel = jnp.take(expert_up, idx, axis=0)
        return jnp.einsum('bthk,bthkd->btd', (g * act).astype(xi.dtype), v_sel)

    out = lax.map(chunk, xc)
    return out.transpose(1, 0, 2, 3).reshape(B, S, D)


def setup_inputs(seed: int = 0) -> dict:
    key = jax.random.key(seed)
    ks = iter(jax.random.split(key, 64))

    def nrm(shape, scale):
        return jax.random.normal(next(ks), shape, jnp.float32) * scale

    def gain(n):
        return 1.0 + 0.1 * jax.random.normal(next(ks), (n,), jnp.float32)

    d = D_MODEL
    inp = {}
    inp['x'] = nrm((BATCH, SEQ, d), 1.0)
    inp['positions'] = jnp.broadcast_to(jnp.arange(SEQ, dtype=jnp.int32), (BATCH, SEQ))
    inp['lb_logits'] = nrm((DEPTH + 1, A_KW), 0.5)
    inp['l0_norm_mix'] = gain(d)
    inp['l0_w_in'] = nrm((d, EVEN_IN), d ** -0.5)
    inp['l0_hgrn_out_norm'] = gain(A_W)
    inp['l0_gmlp_v_norm'] = gain(B_W)
    inp['l0_gmlp_w_s'] = nrm((B_GROUPS, GMLP_CHUNK, GMLP_CHUNK), GMLP_CHUNK ** -0.5)
    inp['l0_gmlp_b_s'] = 1.0 + nrm((B_GROUPS, GMLP_CHUNK), 0.1)
    inp['l0_w_out'] = nrm((A_W + B_W, d), (A_W + B_W) ** -0.5)
    inp['l0_norm_ffn'] = gain(d)
    inp['l0_peer_w_query'] = nrm((d, PEER_HEADS * PEER_QDIM), d ** -0.5)
    inp['l0_peer_sub_keys'] = nrm((PEER_HEADS, 2, PEER_N_KEYS, PEER_HALF), PEER_HALF ** -0.5)
    inp['l0_peer_expert_down'] = nrm((PEER_N_EXPERTS, d), d ** -0.5)
    inp['l0_peer_expert_up'] = nrm((PEER_N_EXPERTS, d), 0.25)
    inp['l1_norm_mix'] = gain(d)
    inp['l1_w_in'] = nrm((d, ODD_IN), d ** -0.5)
    inp['l1_mla_cq_norm'] = gain(Q_LORA)
    inp['l1_mla_ckv_norm'] = gain(KV_LORA)
    inp['l1_mla_w_uq'] = nrm((Q_LORA, C_HEADS * C_QK), Q_LORA ** -0.5)
    inp['l1_mla_w_ukv'] = nrm((KV_LORA, C_HEADS * (C_NOPE + C_V)), KV_LORA ** -0.5)
    inp['l1_mla_q_norm'] = gain(C_QK)
    inp['l1_mla_k_norm'] = gain(C_QK)
    inp['l1_moba_q_norm'] = gain(D_HEAD_DIM)
    inp['l1_moba_k_norm'] = gain(D_HEAD_DIM)
    inp['l1_w_out'] = nrm((C_W + D_W, d), (C_W + D_W) ** -0.5)
    inp['l1_norm_ffn'] = gain(d)
    inp['l1_peer_w_query'] = nrm((d, PEER_HEADS * PEER_QDIM), d ** -0.5)
    inp['l1_peer_sub_keys'] = nrm((PEER_HEADS, 2, PEER_N_KEYS, PEER_HALF), PEER_HALF ** -0.5)
    inp['l1_peer_expert_down'] = nrm((PEER_N_EXPERTS, d), d ** -0.5)
    inp['l1_peer_expert_up'] = nrm((PEER_N_EXPERTS, d), 0.25)
    return inp


def reference(x, positions, lb_logits,
              l0_norm_mix, l0_w_in, l0_hgrn_out_norm, l0_gmlp_v_norm, l0_gmlp_w_s, l0_gmlp_b_s, l0_w_out,
              l0_norm_ffn, l0_peer_w_query, l0_peer_sub_keys, l0_peer_expert_down, l0_peer_expert_up,
              l1_norm_mix, l1_w_in, l1_mla_cq_norm, l1_mla_ckv_norm, l1_mla_w_uq, l1_mla_w_ukv,
              l1_mla_q_norm, l1_mla_k_norm, l1_moba_q_norm, l1_moba_k_norm, l1_w_out,
              l1_norm_ffn, l1_peer_w_query, l1_peer_sub_keys, l1_peer_expert_down, l1_peer_expert_up):
    lbs = hgrn_lower_bounds(lb_logits)
    mixer_params = [
        (lbs[0], l0_norm_mix, l0_w_in, l0_hgrn_out_norm, l0_gmlp_v_norm, l0_gmlp_w_s, l0_gmlp_b_s, l0_w_out),
        (positions, l1_norm_mix, l1_w_in, l1_mla_cq_norm, l1_mla_ckv_norm, l1_mla_w_uq, l1_mla_w_ukv,
         l1_mla_q_norm, l1_mla_k_norm, l1_moba_q_norm, l1_moba_k_norm, l1_w_out),
    ]
    ffn_params = [
        (l0_norm_ffn, l0_peer_w_query, l0_peer_sub_keys, l0_peer_expert_down, l0_peer_expert_up),
        (l1_norm_ffn, l1_peer_w_query, l1_peer_sub_keys, l1_peer_expert_down, l1_peer_expert_up),
    ]
    for layer in range(DEPTH):
        mixer = hgrn_gmlp_mixer if layer % 2 == 0 else mla_moba_mixer
        x = x + mixer(x, *mixer_params[layer])
        x = x + peer_ffn(x, *ffn_params[layer])
    return x
```

```python
import numpy as np
from contextlib import ExitStack
import concourse.bass as bass
import concourse.mybir as mybir
from concourse.bass_utils import run_bass_kernel_spmd

F32 = mybir.dt.float32
BF16 = mybir.dt.bfloat16
I32 = mybir.dt.int32
U32 = mybir.dt.uint32
AF = mybir.ActivationFunctionType
ALU = mybir.AluOpType
AX = mybir.AxisListType

ENGS = ("pe", "act", "dve", "pool", "sp")
RMS_EPS = 1e-6


class _Op:
    __slots__ = ("eng", "fn", "reads", "writes", "dma", "stream", "deps", "signal",
                 "sigval", "ndma", "idx")

    def __init__(self, eng, fn, reads, writes, dma, stream):
        self.eng = eng
        self.fn = fn
        self.reads = reads
        self.writes = writes
        self.dma = dma
        self.stream = stream
        self.deps = {}
        self.signal = False
        self.sigval = None
        self.ndma = 0


class Sched:
    def __init__(self, nc):
        self.nc = nc
        self.ops = []

    def op(self, eng, fn, reads=(), writes=()):
        self.ops.append(_Op(eng, fn, tuple(reads), tuple(writes), False, None))

    def dma(self, eng, fn, reads=(), writes=(), stream=None, n=1):
        o = _Op(eng, fn, tuple(reads), tuple(writes), True, stream)
        o.ndma = n
        self.ops.append(o)

    def wait_all(self, eng, keys):
        self.ops.append(_Op(eng, None, tuple(keys), (), False, None))

    def emit(self, stack):
        nc = self.nc
        ops = self.ops
        writers = {}
        readers = {}
        last_on_stream = {}
        for i, op in enumerate(ops):
            op.idx = i
            deps = {}
            for k in op.reads:
                for w in writers.get(k, ()):
                    deps[w] = True
            for k in op.writes:
                for w in writers.get(k, ()):
                    if w not in deps:
                        deps[w] = False
                for r in readers.get(k, ()):
                    if r not in deps:
                        deps[r] = False
            if op.dma:
                p = last_on_stream.get(op.stream)
                if p is not None and p not in deps:
                    deps[p] = True
                last_on_stream[op.stream] = i
            for k in op.reads:
                readers.setdefault(k, []).append(i)
            for k in op.writes:
                if readers.get(k):
                    writers[k] = [i]
                    readers[k] = []
                else:
                    writers.setdefault(k, []).append(i)
            deps.pop(i, None)
            fd = {}
            for d, raw in deps.items():
                p = ops[d]
                if p.fn is None:
                    continue
                if (not p.dma) and p.eng == op.eng and p.eng == "pe":
                    continue
                fd[d] = raw
                p.signal = True
            op.deps = fd
        cnt = {e: 0 for e in ENGS}
        scnt = {}
        for op in ops:
            if op.fn is None:
                continue
            if op.dma:
                scnt[op.stream] = scnt.get(op.stream, 0) + 16 * op.ndma
                op.sigval = scnt[op.stream]
            elif op.signal:
                cnt[op.eng] += 1
                op.sigval = cnt[op.eng]
        esem = {e: stack.enter_context(nc.semaphore("sem_" + e)) for e in ENGS}
        ssem = {}
        for s in scnt:
            ssem[s] = stack.enter_context(nc.semaphore("dsem_%d" % len(ssem)))
        self.n_sems = len(esem) + len(ssem)
        self.counts = dict(cnt)
        block = stack.enter_context(nc.Block())
        byeng = {e: [o for o in ops if o.eng == e] for e in ENGS}

        def run(eng_name, engine):
            waited = {}
            for op in byeng[eng_name]:
                need = {}
                for d in op.deps:
                    p = ops[d]
                    if p.dma:
                        key = ("s", p.stream)
                        sem = ssem[p.stream]
                    else:
                        key = ("e", p.eng)
                        sem = esem[p.eng]
                    if waited.get(key, 0) >= p.sigval:
                        continue
                    if need.get(key, (None, 0))[1] < p.sigval:
                        need[key] = (sem, p.sigval)
                for key, (sem, val) in need.items():
                    engine.wait_ge(sem, val)
                    waited[key] = val
                if op.fn is None:
                    continue
                if op.dma:
                    op.fn(engine, ssem[op.stream])
                else:
                    ins = op.fn(engine)
                    if op.signal:
                        if isinstance(ins, (list, tuple)):
                            ins = ins[-1]
                        ins.then_inc(esem[op.eng], 1)

        block.tensor(lambda e: run("pe", e))
        block.scalar(lambda e: run("act", e))
        block.vector(lambda e: run("dve", e))
        block.gpsimd(lambda e: run("pool", e))
        block.sync(lambda e: run("sp", e))


class Ctx:
    def __init__(self):
        self.nc = bass.Bass("TRN2", target_bir_lowering=False)
        self.st = ExitStack()
        self.S = Sched(self.nc)
        self._n = 0
        self.PS = self.st.enter_context(self.nc.psum_tensor("PS", [128, 4096], F32))
        self.outs = []

    def din(self, name, shape, dt=F32):
        return self.nc.dram_tensor(name, list(shape), dt, kind="ExternalInput").ap()

    def dout(self, name, shape, dt=F32):
        self.outs.append(name)
        return self.nc.dram_tensor(name, list(shape), dt, kind="ExternalOutput").ap()

    def sb(self, shape, dt=F32, name=None):
        self._n += 1
        return self.st.enter_context(self.nc.sbuf_tensor("s_" + (name or ("t%d" % self._n)), list(shape), dt))

    def bank(self, i, n=1):
        return self.PS[:, i * 512:(i + n) * 512]

    def V(self, fn, r=(), w=()):
        self.S.op("dve", fn, r, w)

    def A(self, fn, r=(), w=()):
        self.S.op("act", fn, r, w)

    def P(self, fn, r=(), w=()):
        self.S.op("pe", fn, r, w)

    def G(self, fn, r=(), w=()):
        self.S.op("pool", fn, r, w)

    def load(self, eng, out, in_, key, stream=None, reads=()):
        self.S.dma(eng, lambda e, s: e.dma_start(out=out, in_=in_).then_inc(s, 16),
                   reads=list(reads), writes=[key], stream=stream or key)

    def store(self, eng, out, in_, rkey, okey, stream=None):
        self.S.dma(eng, lambda e, s: e.dma_start(out=out, in_=in_).then_inc(s, 16),
                   reads=[rkey], writes=[okey], stream=stream or okey)

    def finish(self, final_keys):
        self.S.wait_all("sp", final_keys)
        self.S.emit(self.st)
        self.st.close()
        return self.nc

    def consts(self):
        io = self.sb([128, 128], I32)
        io2 = self.sb([128, 128], I32)
        self.identb = self.sb([128, 128], BF16)
        self.identf = self.sb([128, 128], F32)
        self.iotaF = self.sb([128, 128], F32)
        self.G(lambda e: e.iota(io[:], pattern=[[1, 128]], base=0, channel_multiplier=-1), w=["io"])
        self.G(lambda e: e.iota(io2[:], pattern=[[1, 128]], base=0, channel_multiplier=0), w=["io2"])
        self.V(lambda e: e.tensor_scalar(out=self.identb[:], in0=io[:], scalar1=0.0, scalar2=None, op0=ALU.is_equal),
               r=["io"], w=["identb"])
        self.V(lambda e: e.tensor_scalar(out=self.identf[:], in0=io[:], scalar1=0.0, scalar2=None, op0=ALU.is_equal),
               r=["io"], w=["identf"])
        self.V(lambda e: e.tensor_copy(out=self.iotaF[:], in_=io2[:]), r=["io2"], w=["iotaF"])


TB = 256
NW = 10
LA = 3


def build_token_phase(NT, n_i1=128):
    c = Ctx()
    nc = c.nc
    x_in = c.din("x", [NT, 1024])
    mix = c.din("mix", [NT, 1024])
    w_out = c.din("w_out", [1024, 1024])
    gffn = c.din("gffn", [1, 1024])
    wq = c.din("wq", [16, 128, 1024])
    skT = c.din("skT", [128, 16 * 128])
    dT = c.din("dT", [128, 128, 1024])
    up = c.din("up", [16384, 1024])
    out = c.dout("out", [NT, 1024])
    c.consts()
    NB = NT // TB

    gb = c.sb([128, 1024], F32)
    c.load("sp", gb[:], gffn.to_broadcast([128, 1024]), "gb")
    skb = c.sb([128, 16 * 128], BF16)
    c.load("pool", skb[:], skT, "skb")
    iota16 = c.iotaF[:, 0:16]

    wbuf = [c.sb([128, 1024], BF16, name="wbuf%d" % i) for i in range(NW)]
    wctr = [0]

    def wload(src, eng="pool", reads=()):
        s = wctr[0] % NW
        wctr[0] += 1
        c.load(eng, wbuf[s][:], src, ("w", s), reads=reads)
        return s

    dTb = nc.dram_tensor("dTb", [128, 128, 1024], BF16, kind="Internal").ap()
    upb = nc.dram_tensor("upb", [16384, 1024], BF16, kind="Internal").ap()
    for i1 in range(n_i1):
        c.S.dma("pool", lambda e, s, i1=i1: e.dma_start(out=dTb[i1], in_=dT[i1]).then_inc(s, 16),
                writes=[("dTb", i1)], stream=("pc", (2 * i1) % 8))
        c.S.dma("pool", lambda e, s, i1=i1: e.dma_start(out=upb[i1 * 128:(i1 + 1) * 128, :], in_=up[i1 * 128:(i1 + 1) * 128, :]).then_inc(s, 16),
                writes=[("upb", i1)], stream=("pc", (2 * i1 + 1) % 8))

    xt = [c.sb([128, 1024], F32, name="xt%d" % j) for j in range(2)]
    mt = c.sb([128, 1024], F32, name="mt")
    mb = c.sb([128, 1024], BF16, name="mb")
    mixT = [c.sb([128, 8, 128], BF16, name="mixT%d" % j) for j in range(2)]
    x1 = [c.sb([128, 1024], F32, name="x1_%d" % j) for j in range(2)]
    junk = c.sb([128, 1024], F32, name="junk")
    ssq = c.sb([128, 2], F32, name="ssq")
    rstd = c.sb([128, 2], F32, name="rstd")
    xn = c.sb([128, 1024], BF16, name="xn")
    xnT = c.sb([128, 8, TB], BF16, name="xnT")
    qT = c.sb([128, 16, TB], BF16, name="qT")
    bufA = c.sb([128, 2048], F32, name="bufA")
    bufB = c.sb([128, 2048], F32, name="bufB")
    v = c.sb([128, 16, 16], F32, name="v")
    ix = c.sb([128, 16, 16], U32, name="ix")
    ixf = c.sb([128, 16, 16], F32, name="ixf")
    ts_ = c.sb([128, 8, 16], F32, name="ts")
    tc_ = c.sb([128, 8, 16], U32, name="tc")
    ai = c.sb([128, 8, 16], U32, name="ai")
    bi = c.sb([128, 8, 16], U32, name="bi")
    af = c.sb([128, 8, 16], F32, name="af")
    bf = c.sb([128, 8, 16], F32, name="bf")
    IG = c.sb([128, 3, 128], F32, name="IG")
    dd = c.sb([128, 8, 16], F32, name="dd")
    ee = c.sb([128, 8, 16], F32, name="ee")
    es = c.sb([128, 8], F32, name="es")
    rs = c.sb([128, 8], F32, name="rs")
    IT = c.sb([128, 3, TB], F32, name="IT")
    NS = 4
    At = [c.sb([128, 128], BF16, name="At%d" % i) for i in range(NS)]
    Bt = [c.sb([128, 128], BF16, name="Bt%d" % i) for i in range(NS)]
    GT = c.sb([128, 128, TB], BF16, name="GT")
    sq = [c.sb([128, TB], F32, name="sq%d" % i) for i in range(4)]
    uu = [c.sb([128, TB], F32, name="uu%d" % i) for i in range(4)]
    u2 = [c.sb([128, TB], F32, name="u2%d" % i) for i in range(4)]
    sg = [c.sb([128, TB], F32, name="sg%d" % i) for i in range(4)]
    hg = [c.sb([128, TB], F32, name="hg%d" % i) for i in range(4)]
    AT = [c.sb([128, TB], BF16, name="AT%d" % i) for i in range(4)]
    xo = [c.sb([128, 1024], F32, name="xo%d" % j) for j in range(2)]

    pTb = c.bank(4).bitcast(BF16).rearrange("p (k t) -> p k t", k=8)
    pTf = c.bank(4)[:, 0:384].rearrange("p (k t) -> p k t", k=3)

    for b in range(NB):
        for j in range(2):
            r0 = b * TB + j * 128
            c.load("sp", xt[j][:], x_in[r0:r0 + 128, :], ("xt", j))
            c.load("sp", mt[:], mix[r0:r0 + 128, :], "mt")
            c.A(lambda e: e.copy(out=mb[:], in_=mt[:]), r=["mt"], w=["mb"])
            for k in range(8):
                c.P(lambda e, k=k: e.transpose(out=pTb[:, k, :], in_=mb[:, k * 128:(k + 1) * 128], identity=c.identb[:]),
                    r=["mb", "identb"], w=[("bk", 4)])
            c.V(lambda e, j=j: e.tensor_copy(out=mixT[j][:], in_=pTb), r=[("bk", 4)], w=[("mixT", j)])
        for k in range(8):
            s = wload(w_out[k * 128:(k + 1) * 128, :])
            for j in range(2):
                for hf in range(2):
                    c.P(lambda e, k=k, j=j, hf=hf, s=s: e.matmul(
                        c.bank(j * 2 + hf), lhsT=mixT[j][:, k, :], rhs=wbuf[s][:, hf * 512:(hf + 1) * 512],
                        start=(k == 0), stop=(k == 7)),
                        r=[("mixT", j), ("w", s)], w=[("bk", j * 2 + hf)])
        for j in range(2):
            c.V(lambda e, j=j: e.tensor_tensor(out=x1[j][:], in0=xt[j][:], in1=c.bank(j * 2, 2), op=ALU.add),
                r=[("xt", j), ("bk", j * 2), ("bk", j * 2 + 1)], w=[("x1", j)])
        for j in range(2):
            c.A(lambda e, j=j: e.activation(out=junk[:], in_=x1[j][:], func=AF.Square, accum_out=ssq[:, j:j + 1]),
                r=[("x1", j)], w=["junk", ("ssq", j)])
            c.V(lambda e, j=j: e.tensor_scalar(out=rstd[:, j:j + 1], in0=ssq[:, j:j + 1], scalar1=1.0 / 1024, scalar2=RMS_EPS,
                                               op0=ALU.mult, op1=ALU.add), r=[("ssq", j)], w=[("rstd", j)])
            c.A(lambda e, j=j: e.activation(out=rstd[:, j:j + 1], in_=rstd[:, j:j + 1], func=AF.Sqrt),
                r=[("rstd", j)], w=[("rstd", j)])
            c.V(lambda e, j=j: e.reciprocal(out=rstd[:, j:j + 1], in_=rstd[:, j:j + 1]), r=[("rstd", j)], w=[("rstd", j)])
            c.V(lambda e, j=j: e.scalar_tensor_tensor(out=xn[:], in0=x1[j][:], scalar=rstd[:, j:j + 1], in1=gb[:],
                                                      op0=ALU.mult, op1=ALU.mult),
                r=[("x1", j), ("rstd", j), "gb"], w=["xn"])
            for k in range(8):
                c.P(lambda e, k=k: e.transpose(out=pTb[:, k, :], in_=xn[:, k * 128:(k + 1) * 128], identity=c.identb[:]),
                    r=["xn", "identb"], w=[("bk", 4)])
            c.V(lambda e, j=j: e.tensor_copy(out=xnT[:, :, j * 128:(j + 1) * 128], in_=pTb), r=[("bk", 4)], w=["xnT"])
        for cc in range(16):
            s = wload(wq[cc])
            hb = 5 + (cc % 2)
            for k in range(8):
                c.P(lambda e, k=k, s=s, hb=hb: e.matmul(c.bank(hb)[:, 0:TB], lhsT=wbuf[s][:, k * 128:(k + 1) * 128],
                                                        rhs=xnT[:, k, :], start=(k == 0), stop=(k == 7)),
                    r=[("w", s), "xnT"], w=[("bk", hb)])
            if cc % 2 == 0:
                c.A(lambda e, cc=cc, hb=hb: e.copy(out=qT[:, cc, :], in_=c.bank(hb)[:, 0:TB]), r=[("bk", hb)], w=[("qT", cc)])
            else:
                c.V(lambda e, cc=cc, hb=hb: e.tensor_copy(out=qT[:, cc, :], in_=c.bank(hb)[:, 0:TB]), r=[("bk", hb)], w=[("qT", cc)])
        for j in range(2):
            for cc in range(16):
                c.P(lambda e, cc=cc, j=j: e.matmul(c.bank(0, 4)[:, cc * 128:(cc + 1) * 128], lhsT=qT[:, cc, j * 128:(j + 1) * 128],
                                                   rhs=skb[:, cc * 128:(cc + 1) * 128], start=True, stop=True),
                    r=[("qT", cc), "skb"], w=[("bk", cc // 4)])
            c.V(lambda e, j=j: e.tensor_copy(out=bufA[:], in_=c.bank(0, 4)),
                r=[("bk", 0), ("bk", 1), ("bk", 2), ("bk", 3)], w=["bufA"])
            A3 = bufA[:].rearrange("p (c n) -> p c n", c=16)
            B3 = bufB[:].rearrange("p (c n) -> p c n", c=16)
            for cc in range(16):
                c.V(lambda e, cc=cc: e.max(out=v[:, cc, 0:8], in_=A3[:, cc, :]), r=["bufA"], w=[("v", cc)])
                c.V(lambda e, cc=cc: e.max_index(out=ix[:, cc, 0:8], in_max=v[:, cc, 0:8], in_values=A3[:, cc, :]),
                    r=["bufA", ("v", cc)], w=[("ix", cc)])
                c.V(lambda e, cc=cc: e.match_replace(out=B3[:, cc, :], in_to_replace=v[:, cc, 0:8], in_values=A3[:, cc, :], imm_value=-1e30),
                    r=["bufA", ("v", cc)], w=[("bufB", cc)])
                c.V(lambda e, cc=cc: e.max(out=v[:, cc, 8:16], in_=B3[:, cc, :]), r=[("bufB", cc)], w=[("v2", cc)])
                c.V(lambda e, cc=cc: e.max_index(out=ix[:, cc, 8:16], in_max=v[:, cc, 8:16], in_values=B3[:, cc, :]),
                    r=[("bufB", cc), ("v2", cc)], w=[("ix2", cc)])
            vall = [("v", cc) for cc in range(16)] + [("v2", cc) for cc in range(16)]
            ixall = [("ix", cc) for cc in range(16)] + [("ix2", cc) for cc in range(16)]
            c.V(lambda e: e.tensor_copy(out=ixf[:], in_=ix[:]), r=ixall, w=["ixf"])
            v4 = v[:].rearrange("p (h two) a -> p h two a", two=2)
            ixf4 = ixf[:].rearrange("p (h two) a -> p h two a", two=2)
            cand = bufA[:].rearrange("p (h a b) -> p h a b", h=8, a=16)
            candf = bufA[:].rearrange("p (h n) -> p h n", h=8)
            candB = bufB[:].rearrange("p (h n) -> p h n", h=8)
            c.V(lambda e: e.tensor_tensor(out=cand, in0=v4[:, :, 0, :].unsqueeze(3).to_broadcast([128, 8, 16, 16]),
                                          in1=v4[:, :, 1, :].unsqueeze(2).to_broadcast([128, 8, 16, 16]), op=ALU.add),
                r=vall, w=["bufA"])
            for h in range(8):
                c.V(lambda e, h=h: e.max(out=ts_[:, h, 0:8], in_=candf[:, h, :]), r=["bufA"], w=[("ts", h)])
                c.V(lambda e, h=h: e.max_index(out=tc_[:, h, 0:8], in_max=ts_[:, h, 0:8], in_values=candf[:, h, :]),
                    r=["bufA", ("ts", h)], w=[("tc", h)])
                c.V(lambda e, h=h: e.match_replace(out=candB[:, h, :], in_to_replace=ts_[:, h, 0:8], in_values=candf[:, h, :], imm_value=-1e30),
                    r=["bufA", ("ts", h)], w=[("cB", h)])
                c.V(lambda e, h=h: e.max(out=ts_[:, h, 8:16], in_=candB[:, h, :]), r=[("cB", h)], w=[("ts2", h)])
                c.V(lambda e, h=h: e.max_index(out=tc_[:, h, 8:16], in_max=ts_[:, h, 8:16], in_values=candB[:, h, :]),
                    r=[("cB", h), ("ts2", h)], w=[("tc2", h)])
            tsall = [("ts", h) for h in range(8)] + [("ts2", h) for h in range(8)]
            tcall = [("tc", h) for h in range(8)] + [("tc2", h) for h in range(8)]
            c.V(lambda e: e.tensor_scalar(out=ai[:], in0=tc_[:], scalar1=4, scalar2=None, op0=ALU.logical_shift_right), r=tcall, w=["ai"])
            c.V(lambda e: e.tensor_scalar(out=bi[:], in0=tc_[:], scalar1=15, scalar2=None, op0=ALU.bitwise_and), r=tcall, w=["bi"])
            c.V(lambda e: e.tensor_copy(out=af[:], in_=ai[:]), r=["ai"], w=["af"])
            c.V(lambda e: e.tensor_copy(out=bf[:], in_=bi[:]), r=["bi"], w=["bf"])
            i16b = iota16.unsqueeze(1).unsqueeze(1).to_broadcast([128, 8, 16, 16])
            for which, (sel, dst) in enumerate(((af, 0), (bf, 1))):
                oh = (bufA if which == 0 else bufB)[:].rearrange("p (h k j) -> p h k j", h=8, k=16)
                okey = "bufA" if which == 0 else "ohB"
                rk = ["af"] if which == 0 else ["bf"]
                c.V(lambda e, sel=sel, oh=oh: e.tensor_tensor(out=oh, in0=sel[:].unsqueeze(3).to_broadcast([128, 8, 16, 16]),
                                                              in1=i16b, op=ALU.is_equal),
                    r=rk + ["iotaF"] + tsall + [("cB", h) for h in range(8)], w=[okey])
                c.V(lambda e, which=which, oh=oh: e.tensor_tensor(out=oh, in0=oh,
                                                                  in1=ixf4[:, :, which, :].unsqueeze(2).to_broadcast([128, 8, 16, 16]),
                                                                  op=ALU.mult), r=[okey, "ixf"], w=[okey])
                c.V(lambda e, dst=dst, oh=oh: e.tensor_reduce(out=IG[:, dst, :].rearrange("p (h k) -> p h k", h=8), in_=oh,
                                                              axis=AX.X, op=ALU.add), r=[okey], w=[("IG", dst)])
            c.V(lambda e: e.tensor_tensor(out=dd[:], in0=ts_[:], in1=ts_[:, :, 0:1].to_broadcast([128, 8, 16]), op=ALU.subtract),
                r=tsall, w=["dd"])
            c.A(lambda e: e.activation(out=ee[:], in_=dd[:], func=AF.Exp), r=["dd"], w=["ee"])
            c.V(lambda e: e.tensor_reduce(out=es[:], in_=ee[:], axis=AX.X, op=ALU.add), r=["ee"], w=["es"])
            c.V(lambda e: e.reciprocal(out=rs[:], in_=es[:]), r=["es"], w=["rs"])
            c.V(lambda e: e.tensor_tensor(out=IG[:, 2, :].rearrange("p (h k) -> p h k", h=8), in0=ee[:],
                                          in1=rs[:].unsqueeze(2).to_broadcast([128, 8, 16]), op=ALU.mult),
                r=["ee", "rs"], w=[("IG", 2)])
            for q in range(3):
                c.P(lambda e, q=q: e.transpose(out=pTf[:, q, :], in_=IG[:, q, :], identity=c.identf[:]),
                    r=[("IG", q), "identf"], w=[("bk", 4)])
            c.A(lambda e, j=j: e.copy(out=IT[:, :, j * 128:(j + 1) * 128], in_=pTf), r=[("bk", 4)], w=["IT"])
        for t in range(TB):
            s = t % NS
            gbk = 4 + (t // 4) % 4
            c.V(lambda e, t=t, s=s: e.tensor_scalar(out=At[s][:], in0=c.iotaF[:], scalar1=IT[:, 0, t:t + 1], scalar2=IT[:, 2, t:t + 1],
                                                    op0=ALU.is_equal, op1=ALU.mult), r=["IT", "iotaF"], w=[("At", s)])
            c.V(lambda e, t=t, s=s: e.tensor_scalar(out=Bt[s][:], in0=c.iotaF[:], scalar1=IT[:, 1, t:t + 1], scalar2=None,
                                                    op0=ALU.is_equal), r=["IT", "iotaF"], w=[("Bt", s)])
            c.P(lambda e, t=t, s=s, gbk=gbk: e.matmul(c.bank(gbk)[:, (t % 4) * 128:(t % 4 + 1) * 128], lhsT=Bt[s][:], rhs=At[s][:],
                                                      start=True, stop=True),
                r=[("At", s), ("Bt", s)], w=[("bk", gbk)])
            if t % 4 == 3:
                t0 = t - 3
                c.A(lambda e, t0=t0, gbk=gbk: e.copy(out=GT[:, :, t0:t0 + 4].rearrange("p i t -> p t i"),
                                                     in_=c.bank(gbk).rearrange("p (t i) -> p t i", t=4)),
                    r=[("bk", gbk)], w=["GT"])
        def HT(i1):
            s = wload(dTb[i1], "sp", [("dTb", i1)])
            hb = 4 + (i1 % 4)
            for k in range(8):
                c.P(lambda e, k=k, s=s, hb=hb: e.matmul(c.bank(hb)[:, 0:TB], lhsT=wbuf[s][:, k * 128:(k + 1) * 128],
                                                        rhs=xnT[:, k, :], start=(k == 0), stop=(k == 7)),
                    r=[("w", s), "xnT"], w=[("bk", hb)])

        for i0 in range(min(LA, n_i1)):
            HT(i0)
        for i1 in range(n_i1):
            p = i1 % 4
            hb = 4 + p
            h_ps = c.bank(hb)[:, 0:TB]
            su = wload(upb[i1 * 128:(i1 + 1) * 128, :], "pool", [("upb", i1)])
            if i1 + LA < n_i1:
                HT(i1 + LA)
            c.A(lambda e, p=p, h_ps=h_ps: e.activation(out=sq[p][:], in_=h_ps, func=AF.Square, scale=0.044715 ** 0.5),
                r=[("bk", hb)], w=[("sq", p)])
            c.V(lambda e, p=p, h_ps=h_ps: e.scalar_tensor_tensor(out=u2[p][:], in0=sq[p][:], scalar=1.0, in1=h_ps, op0=ALU.add, op1=ALU.mult),
                r=[("sq", p), ("bk", hb)], w=[("u2", p)])
            c.A(lambda e, p=p: e.activation(out=sg[p][:], in_=u2[p][:], func=AF.Sigmoid, scale=1.5957691216057308),
                r=[("u2", p)], w=[("sg", p)])
            c.V(lambda e, p=p, h_ps=h_ps, i1=i1: e.tensor_tensor(out=hg[p][:], in0=GT[:, i1, :], in1=h_ps, op=ALU.mult),
                r=["GT", ("bk", hb)], w=[("hg", p)])
            c.V(lambda e, p=p: e.tensor_tensor(out=AT[p][:], in0=hg[p][:], in1=sg[p][:], op=ALU.mult),
                r=[("hg", p), ("sg", p)], w=[("AT", p)])
            for j in range(2):
                for hf in range(2):
                    c.P(lambda e, p=p, j=j, hf=hf, su=su, i1=i1: e.matmul(
                        c.bank(j * 2 + hf), lhsT=AT[p][:, j * 128:(j + 1) * 128], rhs=wbuf[su][:, hf * 512:(hf + 1) * 512],
                        start=(i1 == 0), stop=(i1 == n_i1 - 1)),
                        r=[("AT", p), ("w", su)], w=[("bk", j * 2 + hf)])
        for j in range(2):
            r0 = b * TB + j * 128
            c.V(lambda e, j=j: e.tensor_tensor(out=xo[j][:], in0=x1[j][:], in1=c.bank(j * 2, 2), op=ALU.add),
                r=[("x1", j), ("bk", j * 2), ("bk", j * 2 + 1)], w=[("xo", j)])
            c.store("sp", out[r0:r0 + 128, :], xo[j][:], ("xo", j), ("out", b, j), stream=("out", j))
    return c.finish([("out", b, j) for b in range(NB) for j in range(2)])


def token_inputs(x, mix, w_out, g, w_query, sub_keys, down, up):
    wq = np.ascontiguousarray(w_query.reshape(8, 128, 16, 128).transpose(2, 1, 0, 3)).reshape(16, 128, 1024)
    skT = np.ascontiguousarray(sub_keys.reshape(16, 128, 128).transpose(2, 0, 1)).reshape(128, 2048)
    dT = np.ascontiguousarray(down.reshape(128, 128, 8, 128).transpose(0, 3, 2, 1)).reshape(128, 128, 1024)
    return {"x": np.ascontiguousarray(x), "mix": np.ascontiguousarray(mix), "w_out": np.ascontiguousarray(w_out),
            "gffn": np.ascontiguousarray(g.reshape(1, 1024)), "wq": wq, "skT": skT, "dT": dT,
            "up": np.ascontiguousarray(up)}


def norm_transpose_tile(c, x_src, xt, junk, ssq, rstd, xn, gb, xnT_out, pbank, tag):
    pTb = c.bank(pbank).bitcast(BF16).rearrange("p (k t) -> p k t", k=8)
    c.load("sp", xt[:], x_src, (tag, "xt"))
    c.A(lambda e: e.activation(out=junk[:], in_=xt[:], func=AF.Square, accum_out=ssq[:, 0:1]),
        r=[(tag, "xt")], w=["junk", (tag, "ssq")])
    c.V(lambda e: e.tensor_scalar(out=rstd[:, 0:1], in0=ssq[:, 0:1], scalar1=1.0 / 1024, scalar2=RMS_EPS,
                                  op0=ALU.mult, op1=ALU.add), r=[(tag, "ssq")], w=[(tag, "rstd")])
    c.A(lambda e: e.activation(out=rstd[:, 0:1], in_=rstd[:, 0:1], func=AF.Sqrt), r=[(tag, "rstd")], w=[(tag, "rstd")])
    c.V(lambda e: e.reciprocal(out=rstd[:, 0:1], in_=rstd[:, 0:1]), r=[(tag, "rstd")], w=[(tag, "rstd")])
    c.V(lambda e: e.scalar_tensor_tensor(out=xn[:], in0=xt[:], scalar=rstd[:, 0:1], in1=gb[:], op0=ALU.mult, op1=ALU.mult),
        r=[(tag, "xt"), (tag, "rstd"), "gb"], w=[(tag, "xn")])
    for k in range(8):
        c.P(lambda e, k=k: e.transpose(out=pTb[:, k, :], in_=xn[:, k * 128:(k + 1) * 128], identity=c.identb[:]),
            r=[(tag, "xn"), "identb"], w=[("bk", pbank)])
    c.V(lambda e: e.tensor_copy(out=xnT_out, in_=pTb), r=[("bk", pbank)], w=[(tag, "xnT")])


def gelu_tanh(c, out, in_, n, tmp, rkeys, wkey, tag, extra_mul=None):
    sq, uu, sg = tmp
    c.A(lambda e: e.activation(out=sq, in_=in_, func=AF.Square), r=rkeys, w=[(tag, "sq")])
    c.V(lambda e: e.tensor_scalar(out=uu, in0=sq, scalar1=0.044715, scalar2=1.0, op0=ALU.mult, op1=ALU.add),
        r=[(tag, "sq")], w=[(tag, "uu")])
    c.V(lambda e: e.tensor_tensor(out=uu, in0=uu, in1=in_, op=ALU.mult), r=[(tag, "uu")] + list(rkeys), w=[(tag, "uu")])
    c.A(lambda e: e.activation(out=sg, in_=uu, func=AF.Sigmoid, scale=1.5957691216057308), r=[(tag, "uu")], w=[(tag, "sg")])
    c.V(lambda e: e.tensor_tensor(out=out, in0=sg, in1=in_, op=ALU.mult), r=[(tag, "sg")] + list(rkeys), w=[wkey])


def build_l0_mixer(S_len):
    c = Ctx()
    x_in = c.din("x", [S_len, 1024])
    gmix = c.din("gmix", [1, 1024])
    w6 = c.din("w6", [1024, 768])
    lbl = c.din("lbl", [1, 384])
    gO_d = c.din("gO", [1, 128])
    gV_d = c.din("gV", [1, 128])
    wsT_d = c.din("wsT", [128, 128])
    bs_d = c.din("bs", [128, 1])
    out = c.dout("out", [S_len, 256])
    c.consts()
    NTL = S_len // 128

    gb = c.sb([128, 1024], F32, name="gb")
    c.load("sp", gb[:], gmix.to_broadcast([128, 1024]), "gb")
    w6b = c.sb([128, 8, 768], BF16, name="w6b")
    c.load("pool", w6b[:], w6.rearrange("(k p) n -> p k n", p=128), "w6b")
    gO = c.sb([128, 128], F32, name="gO")
    c.load("sp", gO[:], gO_d.to_broadcast([128, 128]), "gO")
    gV = c.sb([128, 128], F32, name="gV")
    c.load("sp", gV[:], gV_d.to_broadcast([128, 128]), "gV")
    bs = c.sb([128, 1], F32, name="bs")
    c.load("sp", bs[:], bs_d, "bs")
    wsT = c.sb([128, 128], F32, name="wsT")
    c.load("sp", wsT[:], wsT_d, "wsT")
    ll = c.sb([128, 3, 128], F32, name="ll")
    c.load("sp", ll[:].rearrange("p a n -> p (a n)"), lbl.to_broadcast([128, 384]), "ll")

    io = c.sb([128, 128], I32, name="iomask")
    c.G(lambda e: e.iota(io[:], pattern=[[1, 128]], base=0, channel_multiplier=-1), w=["iomask"])
    LT = c.sb([128, 128], F32, name="LT")
    Lblk = c.sb([128, 128], F32, name="Lblk")
    Ust = c.sb([128, 128], F32, name="Ust")
    cind = c.sb([128, 2], F32, name="cind")
    c.V(lambda e: e.tensor_scalar(out=LT[:], in0=io[:], scalar1=0.0, scalar2=None, op0=ALU.is_ge), r=["iomask"], w=["LT"])
    c.V(lambda e: e.tensor_scalar(out=Lblk[:], in0=io[:], scalar1=0.0, scalar2=None, op0=ALU.is_ge), r=["iomask"], w=["Lblk"])
    c.V(lambda e: e.memset(Lblk[0:64, 64:128], 0.0), w=["Lblk"])
    c.V(lambda e: e.tensor_scalar(out=Ust[:], in0=io[:], scalar1=0.0, scalar2=None, op0=ALU.is_lt), r=["iomask"], w=["Ust"])
    c.V(lambda e: e.memset(Ust[64:128, 0:64], 0.0), w=["Ust"])
    c.V(lambda e: e.memset(cind[:], 0.0), w=["cind"])
    c.V(lambda e: e.memset(cind[0:64, 0:1], 1.0), w=["cind"])
    c.V(lambda e: e.memset(cind[64:128, 1:2], 1.0), w=["cind"])
    WcT = c.sb([128, 128], BF16, name="WcT")
    c.V(lambda e: e.tensor_tensor(out=WcT[:], in0=wsT[:], in1=LT[:], op=ALU.mult), r=["wsT", "LT"], w=["WcT"])
    mx = c.sb([128, 128], F32, name="lbmx")
    lb = c.sb([128, 128], F32, name="lb")
    oml = c.sb([128, 128], F32, name="oml")
    c.V(lambda e: e.tensor_tensor(out=mx[:], in0=ll[:, 0, :], in1=ll[:, 1, :], op=ALU.max), r=["ll"], w=["lbmx"])
    c.V(lambda e: e.tensor_tensor(out=mx[:], in0=mx[:], in1=ll[:, 2, :], op=ALU.max), r=["ll", "lbmx"], w=["lbmx"])
    c.V(lambda e: e.tensor_tensor(out=ll[:], in0=ll[:], in1=mx[:].unsqueeze(1).to_broadcast([128, 3, 128]), op=ALU.subtract),
        r=["ll", "lbmx"], w=["ll"])
    c.A(lambda e: e.activation(out=ll[:], in_=ll[:], func=AF.Exp), r=["ll"], w=["ll"])
    c.V(lambda e: e.tensor_tensor(out=mx[:], in0=ll[:, 0, :], in1=ll[:, 1, :], op=ALU.add), r=["ll"], w=["lbmx"])
    c.V(lambda e: e.tensor_tensor(out=mx[:], in0=mx[:], in1=ll[:, 2, :], op=ALU.add), r=["ll", "lbmx"], w=["lbmx"])
    c.V(lambda e: e.reciprocal(out=mx[:], in_=mx[:]), r=["lbmx"], w=["lbmx"])
    c.V(lambda e: e.tensor_tensor(out=lb[:], in0=ll[:, 0, :], in1=mx[:], op=ALU.mult), r=["ll", "lbmx"], w=["lb"])
    c.V(lambda e: e.tensor_scalar(out=oml[:], in0=lb[:], scalar1=-1.0, scalar2=1.0, op0=ALU.mult, op1=ALU.add), r=["lb"], w=["oml"])

    St = c.sb([128, 128], F32, name="St")
    Sb = [c.sb([128, 128], BF16, name="Sb%d" % i) for i in range(2)]
    c.V(lambda e: e.memset(St[:], 0.0), w=["St"])
    c.V(lambda e: e.memset(Sb[0][:], 0.0), w=[("Sb", 0)])
    qeT0 = [c.sb([128, 128], BF16, name="qeT0_%d" % i) for i in range(2)]
    qeT1 = [c.sb([128, 128], BF16, name="qeT1_%d" % i) for i in range(2)]
    for i in range(2):
        c.V(lambda e, i=i: e.memset(qeT0[i][:], 0.0), w=[("qeT0", i)])
        c.V(lambda e, i=i: e.memset(qeT1[i][:], 0.0), w=[("qeT1", i)])

    def dbl(shape, dt, name):
        return [c.sb(shape, dt, name="%s_%d" % (name, i)) for i in range(2)]

    xt = dbl([128, 1024], F32, "xt")
    junk = c.sb([128, 1024], F32, name="junk")
    ssq = dbl([128, 1], F32, "ssq")
    rstd = dbl([128, 1], F32, "rstd")
    xn = dbl([128, 1024], BF16, "xn")
    xnT = dbl([128, 8, 128], BF16, "xnT")
    sig = dbl([128, 128], F32, "sig")
    ff = dbl([128, 128], F32, "ff")
    gg = dbl([128, 128], F32, "gg")
    kk = dbl([128, 128], F32, "kk")
    vs = dbl([128, 128], BF16, "vs")
    eb = dbl([128, 128], F32, "eb")
    qe = dbl([128, 128], BF16, "qe")
    ke = dbl([128, 128], BF16, "ke")
    kd = dbl([128, 128], BF16, "kd")
    dec = dbl([128, 2], F32, "dec")
    qeTf = dbl([128, 128], BF16, "qeTf")
    keT = dbl([128, 128], BF16, "keT")
    scm = dbl([128, 128], BF16, "scm")
    so = dbl([128, 128], F32, "so")
    osq = dbl([128, 1], F32, "osq")
    t1 = dbl([128, 128], F32, "t1")
    otile = dbl([128, 256], F32, "otile")
    gl = dbl([128, 256], F32, "gl")
    gt0 = dbl([128, 256], F32, "gt0")
    gt1 = dbl([128, 256], F32, "gt1")
    gt2 = dbl([128, 256], F32, "gt2")
    vsq = dbl([128, 1], F32, "vsq")
    vn = dbl([128, 128], BF16, "vn")
    junk2 = c.sb([128, 128], F32, name="junk2")

    for ti in range(NTL):
        p = ti % 2
        T = ("t", p)
        norm_transpose_tile(c, x_in[ti * 128:(ti + 1) * 128, :], xt[p], junk, ssq[p], rstd[p], xn[p], gb, xnT[p][:], 2, T)
        for k in range(8):
            c.P(lambda e, k=k, p=p: e.matmul(c.bank(0), lhsT=xnT[p][:, k, :], rhs=w6b[:, k, 0:512], start=(k == 0), stop=(k == 7)),
                r=[(T, "xnT"), "w6b"], w=[("bk", 0)])
        for k in range(8):
            c.P(lambda e, k=k, p=p: e.matmul(c.bank(1)[:, 0:256], lhsT=xnT[p][:, k, :], rhs=w6b[:, k, 512:768], start=(k == 0), stop=(k == 7)),
                r=[(T, "xnT"), "w6b"], w=[("bk", 1)])
        z0 = c.bank(0)
        z1 = c.bank(1)
        zq, zfg, zin, zog = z0[:, 0:128], z0[:, 128:256], z0[:, 256:384], z0[:, 384:512]
        c.A(lambda e, p=p: e.activation(out=sig[p][:], in_=zfg, func=AF.Sigmoid), r=[("bk", 0)], w=[(T, "sig")])
        c.V(lambda e, p=p: e.tensor_tensor(out=ff[p][:], in0=sig[p][:], in1=oml[:], op=ALU.mult), r=[(T, "sig"), "oml"], w=[(T, "ff")])
        c.V(lambda e, p=p: e.tensor_tensor(out=ff[p][:], in0=ff[p][:], in1=lb[:], op=ALU.add), r=[(T, "ff"), "lb"], w=[(T, "ff")])
        c.A(lambda e, p=p: e.activation(out=gg[p][:], in_=ff[p][:], func=AF.Ln), r=[(T, "ff")], w=[(T, "gg")])
        c.V(lambda e, p=p: e.tensor_scalar(out=kk[p][:], in0=ff[p][:], scalar1=-1.0, scalar2=1.0, op0=ALU.mult, op1=ALU.add),
            r=[(T, "ff")], w=[(T, "kk")])
        c.A(lambda e, p=p: e.activation(out=vs[p][:], in_=zin, func=AF.Silu), r=[("bk", 0)], w=[(T, "vs")])
        c.A(lambda e, p=p: e.activation(out=so[p][:], in_=zog, func=AF.Silu), r=[("bk", 0)], w=[(T, "so")])
        bk3 = c.bank(3)
        c.P(lambda e, p=p: e.matmul(bk3[:, 0:128], lhsT=Lblk[:], rhs=gg[p][:], start=True, stop=True), r=["Lblk", (T, "gg")], w=[("bk", 3)])
        c.P(lambda e, p=p: e.matmul(bk3[:, 128:256], lhsT=Ust[:], rhs=gg[p][:], start=True, stop=True), r=["Ust", (T, "gg")], w=[("bk", 3)])
        c.P(lambda e, p=p: e.matmul(bk3[:, 256:258], lhsT=gg[p][:], rhs=cind[:], start=True, stop=True), r=["cind", (T, "gg")], w=[("bk", 3)])
        c.A(lambda e, p=p: e.activation(out=eb[p][:], in_=bk3[:, 0:128], func=AF.Exp), r=[("bk", 3)], w=[(T, "eb")])
        c.V(lambda e, p=p: e.tensor_tensor(out=qe[p][:], in0=eb[p][:], in1=zq, op=ALU.mult), r=[(T, "eb"), ("bk", 0)], w=[(T, "qe")])
        c.A(lambda e, p=p: e.activation(out=eb[p][:], in_=bk3[:, 0:128], func=AF.Exp, scale=-1.0), r=[("bk", 3), (T, "qe")], w=[(T, "eb")])
        c.V(lambda e, p=p: e.tensor_tensor(out=ke[p][:], in0=eb[p][:], in1=kk[p][:], op=ALU.mult), r=[(T, "eb"), (T, "kk")], w=[(T, "ke")])
        c.A(lambda e, p=p: e.activation(out=eb[p][:], in_=bk3[:, 128:256], func=AF.Exp), r=[("bk", 3), (T, "ke")], w=[(T, "eb")])
        c.V(lambda e, p=p: e.tensor_tensor(out=kd[p][:], in0=eb[p][:], in1=kk[p][:], op=ALU.mult), r=[(T, "eb"), (T, "kk")], w=[(T, "kd")])
        c.A(lambda e, p=p: e.activation(out=dec[p][:], in_=bk3[:, 256:258], func=AF.Exp), r=[("bk", 3)], w=[(T, "dec")])
        pq = c.bank(4).bitcast(BF16)
        c.P(lambda e, p=p: e.transpose(out=pq[:, 0:128], in_=qe[p][:], identity=c.identb[:]), r=[(T, "qe"), "identb"], w=[("bk", 4)])
        c.P(lambda e, p=p: e.transpose(out=pq[:, 128:256], in_=ke[p][:], identity=c.identb[:]), r=[(T, "ke"), "identb"], w=[("bk", 4)])
        c.V(lambda e, p=p: e.tensor_copy(out=qeTf[p][:], in_=pq[:, 0:128]), r=[("bk", 4)], w=[(T, "qeTf")])
        c.A(lambda e, p=p: e.copy(out=qeT0[p][:, 0:64], in_=pq[:, 0:64]), r=[("bk", 4)], w=[("qeT0", p)])
        c.A(lambda e, p=p: e.copy(out=qeT1[p][:, 64:128], in_=pq[:, 64:128]), r=[("bk", 4)], w=[("qeT1", p)])
        c.V(lambda e, p=p: e.tensor_copy(out=keT[p][:], in_=pq[:, 128:256]), r=[("bk", 4)], w=[(T, "keT")])
        c.P(lambda e, p=p: e.matmul(c.bank(5)[:, 0:128], lhsT=keT[p][:], rhs=qeTf[p][:], start=True, stop=True),
            r=[(T, "keT"), (T, "qeTf")], w=[("bk", 5)])
        c.V(lambda e, p=p: e.tensor_tensor(out=scm[p][:], in0=Lblk[:], in1=c.bank(5)[:, 0:128], op=ALU.mult),
            r=["Lblk", ("bk", 5)], w=[(T, "scm")])
        c.P(lambda e, p=p: e.matmul(c.bank(7)[:, 0:128], lhsT=kd[p][0:64, :], rhs=vs[p][0:64, :], start=True, stop=True),
            r=[(T, "kd"), (T, "vs")], w=[("bk", 7)])
        c.V(lambda e, p=p: e.scalar_tensor_tensor(out=St[:], in0=St[:], scalar=dec[p][:, 0:1], in1=c.bank(7)[:, 0:128],
                                                  op0=ALU.mult, op1=ALU.add), r=["St", (T, "dec"), ("bk", 7)], w=["St"])
        c.A(lambda e: e.copy(out=Sb[1][:], in_=St[:]), r=["St"], w=[("Sb", 1)])
        c.P(lambda e, p=p: e.matmul(c.bank(6)[:, 0:128], lhsT=scm[p][:], rhs=vs[p][:], start=True, stop=False),
            r=[(T, "scm"), (T, "vs")], w=[("bk", 6)])
        c.P(lambda e, p=p: e.matmul(c.bank(6)[:, 0:128], lhsT=qeT0[p][:], rhs=Sb[0][:], start=False, stop=False),
            r=[("qeT0", p), ("Sb", 0)], w=[("bk", 6)])
        c.P(lambda e, p=p: e.matmul(c.bank(6)[:, 0:128], lhsT=qeT1[p][:], rhs=Sb[1][:], start=False, stop=True),
            r=[("qeT1", p), ("Sb", 1)], w=[("bk", 6)])
        c.P(lambda e, p=p: e.matmul(c.bank(5)[:, 128:256], lhsT=kd[p][64:128, :], rhs=vs[p][64:128, :], start=True, stop=True),
            r=[(T, "kd"), (T, "vs")], w=[("bk", 5)])
        c.V(lambda e, p=p: e.scalar_tensor_tensor(out=St[:], in0=St[:], scalar=dec[p][:, 1:2], in1=c.bank(5)[:, 128:256],
                                                  op0=ALU.mult, op1=ALU.add), r=["St", (T, "dec"), ("bk", 5)], w=["St"])
        c.A(lambda e: e.copy(out=Sb[0][:], in_=St[:]), r=["St"], w=[("Sb", 0)])
        o_ps = c.bank(6)[:, 0:128]
        c.A(lambda e, p=p: e.activation(out=junk2[:], in_=o_ps, func=AF.Square, accum_out=osq[p][:]), r=[("bk", 6)], w=["junk2", (T, "osq")])
        c.V(lambda e, p=p: e.tensor_scalar(out=osq[p][:], in0=osq[p][:], scalar1=1.0 / 128, scalar2=RMS_EPS, op0=ALU.mult, op1=ALU.add),
            r=[(T, "osq")], w=[(T, "osq")])
        c.A(lambda e, p=p: e.activation(out=osq[p][:], in_=osq[p][:], func=AF.Sqrt), r=[(T, "osq")], w=[(T, "osq")])
        c.V(lambda e, p=p: e.reciprocal(out=osq[p][:], in_=osq[p][:]), r=[(T, "osq")], w=[(T, "osq")])
        c.V(lambda e, p=p: e.scalar_tensor_tensor(out=t1[p][:], in0=o_ps, scalar=osq[p][:, 0:1], in1=gO[:], op0=ALU.mult, op1=ALU.mult),
            r=[("bk", 6), (T, "osq"), "gO"], w=[(T, "t1")])
        c.V(lambda e, p=p: e.tensor_tensor(out=otile[p][:, 0:128], in0=t1[p][:], in1=so[p][:], op=ALU.mult),
            r=[(T, "t1"), (T, "so")], w=[(T, "oa")])
        gelu_tanh(c, gl[p][:], z1[:, 0:256], 256, (gt0[p][:], gt1[p][:], gt2[p][:]), [("bk", 1)], (T, "gl"), (T, "g"))
        c.A(lambda e, p=p: e.activation(out=junk2[:], in_=gl[p][:, 128:256], func=AF.Square, accum_out=vsq[p][:]),
            r=[(T, "gl")], w=["junk2", (T, "vsq")])
        c.V(lambda e, p=p: e.tensor_scalar(out=vsq[p][:], in0=vsq[p][:], scalar1=1.0 / 128, scalar2=RMS_EPS, op0=ALU.mult, op1=ALU.add),
            r=[(T, "vsq")], w=[(T, "vsq")])
        c.A(lambda e, p=p: e.activation(out=vsq[p][:], in_=vsq[p][:], func=AF.Sqrt), r=[(T, "vsq")], w=[(T, "vsq")])
        c.V(lambda e, p=p: e.reciprocal(out=vsq[p][:], in_=vsq[p][:]), r=[(T, "vsq")], w=[(T, "vsq")])
        c.V(lambda e, p=p: e.scalar_tensor_tensor(out=vn[p][:], in0=gl[p][:, 128:256], scalar=vsq[p][:, 0:1], in1=gV[:],
                                                  op0=ALU.mult, op1=ALU.mult), r=[(T, "gl"), (T, "vsq"), "gV"], w=[(T, "vn")])
        c.P(lambda e, p=p: e.matmul(c.bank(7)[:, 256:384], lhsT=WcT[:], rhs=vn[p][:], start=True, stop=True),
            r=["WcT", (T, "vn")], w=[("bk", 7)])
        c.V(lambda e, p=p: e.scalar_tensor_tensor(out=otile[p][:, 128:256], in0=c.bank(7)[:, 256:384], scalar=bs[:, 0:1],
                                                  in1=gl[p][:, 0:128], op0=ALU.add, op1=ALU.mult),
            r=[("bk", 7), "bs", (T, "gl")], w=[(T, "ob")])
        c.S.dma("sp", lambda e, s, p=p, ti=ti: e.dma_start(out=out[ti * 128:(ti + 1) * 128, :], in_=otile[p][:]).then_inc(s, 16),
                reads=[(T, "oa"), (T, "ob")], writes=[("out", ti)], stream=("out", p))
    return c.finish([("out", ti) for ti in range(NTL)])


def l0_inputs(xb, norm_mix, w_in, lb_logits, hgrn_out_norm, gmlp_v_norm, gmlp_w_s, gmlp_b_s, h):
    cols = np.concatenate([np.arange(h * 128, (h + 1) * 128) + off for off in (0, 512, 1024, 1536, 2048, 2560)])
    return {"x": np.ascontiguousarray(xb), "gmix": np.ascontiguousarray(norm_mix.reshape(1, 1024)),
            "w6": np.ascontiguousarray(w_in[:, cols]),
            "lbl": np.ascontiguousarray(lb_logits[:, h * 128:(h + 1) * 128]).reshape(1, 384),
            "gO": np.ascontiguousarray(hgrn_out_norm[h * 128:(h + 1) * 128]).reshape(1, 128),
            "gV": np.ascontiguousarray(gmlp_v_norm[h * 128:(h + 1) * 128]).reshape(1, 128),
            "wsT": np.ascontiguousarray(gmlp_w_s[h].T), "bs": np.ascontiguousarray(gmlp_b_s[h]).reshape(128, 1)}


def rms_rows(c, src, n, gain, out, junk, st, rk, tag, wkey):
    c.A(lambda e: e.activation(out=junk, in_=src, func=AF.Square, accum_out=st), r=rk, w=["junk", (tag, "st")])
    c.V(lambda e: e.tensor_scalar(out=st, in0=st, scalar1=1.0 / n, scalar2=RMS_EPS, op0=ALU.mult, op1=ALU.add),
        r=[(tag, "st")], w=[(tag, "st")])
    c.A(lambda e: e.activation(out=st, in_=st, func=AF.Sqrt), r=[(tag, "st")], w=[(tag, "st")])
    c.V(lambda e: e.reciprocal(out=st, in_=st), r=[(tag, "st")], w=[(tag, "st")])
    c.V(lambda e: e.scalar_tensor_tensor(out=out, in0=src, scalar=st, in1=gain, op0=ALU.mult, op1=ALU.mult),
        r=list(rk) + [(tag, "st"), "gains"], w=[wkey])


SUB = [9]


def build_l1_mixer(S_len, stage=9):
    c = Ctx()
    x_in = c.din("x", [S_len, 1024])
    gmix = c.din("gmix", [1, 1024])
    w832 = c.din("w832", [1024, 832])
    gains_d = c.din("gains", [1, 1024])
    wuq_d = c.din("wuq", [256, 192])
    wukv_d = c.din("wukv", [128, 256])
    pos_d = c.din("pos", [128, S_len // 128], I32)
    invf_d = c.din("invf", [1, 32])
    out = c.dout("out", [S_len, 256])
    c.consts()
    NTL = S_len // 128
    NQB = S_len // 512
    NBLK = S_len // 256
    BIG = 1e30

    gb = c.sb([128, 1024], F32, name="gb")
    c.load("sp", gb[:], gmix.to_broadcast([128, 1024]), "gb")
    gains = c.sb([128, 1024], F32, name="gains")
    c.load("sp", gains[:], gains_d.to_broadcast([128, 1024]), "gains")
    g_cq, g_ckv, g_q, g_k, g_mq, g_mk = (gains[:, 0:256], gains[:, 256:384], gains[:, 384:576], gains[:, 576:768],
                                         gains[:, 768:896], gains[:, 896:1024])
    wb = c.sb([128, 8, 832], BF16, name="wb")
    c.load("pool", wb[:], w832.rearrange("(k p) n -> p k n", p=128), "wb")
    wuq = c.sb([128, 2, 192], BF16, name="wuq")
    c.load("pool", wuq[:], wuq_d.rearrange("(k p) n -> p k n", p=128), "wuq")
    wukv = c.sb([128, 256], BF16, name="wukv")
    c.load("pool", wukv[:], wukv_d, "wukv")
    posi = c.sb([128, NTL], I32, name="posi")
    c.load("sp", posi[:], pos_d, "posi")
    invf = c.sb([128, 32], F32, name="invf")
    c.load("sp", invf[:], invf_d.to_broadcast([128, 32]), "invf")
    posf = c.sb([128, NTL], F32, name="posf")
    c.V(lambda e: e.tensor_copy(out=posf[:], in_=posi[:]), r=["posi"], w=["posf"])
    sinT = c.sb([128, NTL, 32], F32, name="sinT")
    cosT = c.sb([128, NTL, 32], F32, name="cosT")
    rtmp = c.sb([128, NTL, 32], F32, name="rtmp")
    ni = c.sb([128, NTL, 32], I32, name="ni")
    TWO_PI = 6.283185307179586
    C1 = 6.28125
    C2 = TWO_PI - C1
    c.V(lambda e: e.tensor_tensor(out=rtmp[:], in0=posf[:].unsqueeze(2).to_broadcast([128, NTL, 32]),
                                  in1=invf[:].unsqueeze(1).to_broadcast([128, NTL, 32]), op=ALU.mult), r=["posf", "invf"], w=["ang"])
    for tab, shift, key in ((sinT, 0.0, "sinT"), (cosT, 0.5 * np.pi, "cosT")):
        c.V(lambda e, tab=tab, shift=shift: e.tensor_scalar(out=tab[:], in0=rtmp[:], scalar1=float(shift), scalar2=1.0 / TWO_PI,
                                                            op0=ALU.add, op1=ALU.mult), r=["ang"], w=[key])
        c.V(lambda e, tab=tab: e.tensor_copy(out=ni[:], in_=tab[:]), r=[key], w=["ni"])
        c.V(lambda e, tab=tab: e.tensor_copy(out=tab[:], in_=ni[:]), r=["ni"], w=[key])
        c.V(lambda e, tab=tab: e.scalar_tensor_tensor(out=ni[:].bitcast(F32), in0=tab[:], scalar=-C1, in1=rtmp[:], op0=ALU.mult, op1=ALU.add),
            r=[key, "ang"], w=["ni"])
        c.V(lambda e, tab=tab: e.scalar_tensor_tensor(out=tab[:], in0=tab[:], scalar=-C2, in1=ni[:].bitcast(F32), op0=ALU.mult, op1=ALU.add),
            r=[key, "ni"], w=[key])
        c.V(lambda e, tab=tab, shift=shift: e.tensor_scalar(out=tab[:], in0=tab[:], scalar1=float(shift), scalar2=None, op0=ALU.add),
            r=[key], w=[key])
        c.V(lambda e, tab=tab: e.tensor_scalar(out=ni[:].bitcast(F32), in0=tab[:], scalar1=float(np.pi), scalar2=-TWO_PI, op0=ALU.is_gt, op1=ALU.mult),
            r=[key], w=["ni"])
        c.V(lambda e, tab=tab: e.tensor_tensor(out=tab[:], in0=tab[:], in1=ni[:].bitcast(F32), op=ALU.add), r=[key, "ni"], w=[key])
        c.V(lambda e, tab=tab: e.tensor_scalar(out=ni[:].bitcast(F32), in0=tab[:], scalar1=-float(np.pi), scalar2=TWO_PI, op0=ALU.is_lt, op1=ALU.mult),
            r=[key], w=["ni"])
        c.V(lambda e, tab=tab: e.tensor_tensor(out=tab[:], in0=tab[:], in1=ni[:].bitcast(F32), op=ALU.add), r=[key, "ni"], w=[key])
        c.V(lambda e, tab=tab: e.tensor_scalar(out=tab[:], in0=tab[:], scalar1=3.1415925, scalar2=-3.1415925, op0=ALU.min, op1=ALU.max),
            r=[key], w=[key])
        c.A(lambda e, tab=tab: e.activation(out=tab[:], in_=tab[:], func=AF.Sin), r=[key], w=[key])
    io = c.sb([128, 512], I32, name="iom")
    maskD = c.sb([128, 4, 512], BF16, name="maskD")
    for m in range(4):
        c.G(lambda e, m=m: e.iota(io[:], pattern=[[1, 512]], base=-128 * m, channel_multiplier=-1), w=["iom"])
        c.V(lambda e, m=m: e.tensor_scalar(out=maskD[:, m, :], in0=io[:], scalar1=0.0, scalar2=None, op0=ALU.is_ge), r=["iom"], w=["maskD"])
    ioe = c.sb([128, 32], I32, name="ioe")
    Eblk = c.sb([128, 32, 128], BF16, name="Eblk")
    c.G(lambda e: e.iota(ioe[:], pattern=[[-1, 32]], base=0, channel_multiplier=1), w=["ioe"])
    c.V(lambda e: e.tensor_scalar(out=Eblk[:], in0=ioe[:].unsqueeze(2).to_broadcast([128, 32, 128]), scalar1=0.0, scalar2=None,
                                  op0=ALU.is_equal), r=["ioe"], w=["Eblk"])
    ones256 = c.sb([128, 1], F32, name="ones256")
    c.V(lambda e: e.memset(ones256[:], 1.0 / 256), w=["ones256"])
    kcTa = c.sb([128, S_len], BF16, name="kcTa")
    kcTb = c.sb([128, S_len], BF16, name="kcTb")
    kdT = c.sb([128, S_len], BF16, name="kdT")
    Vc = c.sb([128, NTL, 129], BF16, name="Vc")
    Vd = c.sb([128, NTL, 129], BF16, name="Vd")
    c.G(lambda e: e.memset(Vc[:, :, 128:129], 1.0), w=["Vc"])
    c.G(lambda e: e.memset(Vd[:, :, 128:129], 1.0), w=["Vd"])
    kmT = c.sb([128, 32], F32, name="kmT")
    c.V(lambda e: e.memset(kmT[:], 0.0), w=["kmT"])
    qa = c.sb([128, 512], BF16, name="qa")
    qb = c.sb([128, 512], BF16, name="qb")
    qd = c.sb([128, 512], BF16, name="qd")
    MT = c.sb([128, 512], BF16, name="MT")
    xt = c.sb([128, 1024], F32, name="xt")
    junk = c.sb([128, 1024], F32, name="junk")
    ssq = c.sb([128, 1], F32, name="ssq")
    rstd = c.sb([128, 1], F32, name="rstd")
    xn = c.sb([128, 1024], BF16, name="xn")
    xnT = c.sb([128, 8, 128], BF16, name="xnT")
    st = c.sb([128, 8], F32, name="st")
    cqn = c.sb([128, 256], BF16, name="cqn")
    cqT = c.sb([128, 2, 128], BF16, name="cqT")
    ckvn = c.sb([128, 128], BF16, name="ckvn")
    ckvT = c.sb([128, 128], BF16, name="ckvT")
    qk = c.sb([128, 2, 192], F32, name="qk")
    kcat = c.sb([128, 192], F32, name="kcat")
    qkb = c.sb([128, 2, 256], BF16, name="qkb")
    c.V(lambda e: e.memset(qkb[:], 0.0), w=["qkb0", "qkb1", "qkb2"])
    rt = c.sb([128, 4, 2, 32], F32, name="rt")
    qdn = c.sb([128, 128], F32, name="qdn")
    qdb = c.sb([128, 128], BF16, name="qdb")
    kdn = c.sb([128, 128], F32, name="kdn")
    kdb = c.sb([128, 128], BF16, name="kdb")
    qdTf = c.sb([128, 128], F32, name="qdTf")
    gsb = c.sb([128, 32], F32, name="gsb")
    m8 = c.sb([128, 8], F32, name="m8")
    self_ = c.sb([128, 32], F32, name="self")
    Mb = c.sb([128, 128], BF16, name="Mb")
    c.V(lambda e: e.memset(Mb[:], 0.0), w=["Mb"])
    PT = [c.sb([128, 512], BF16, name="PT%d" % i) for i in range(2)]
    rec = c.sb([128, 1], F32, name="rec")
    ktmp = c.sb([128, 1], F32, name="ktmp")
    otile = [c.sb([128, 256], F32, name="otile%d" % i) for i in range(4)]
    pTb = c.bank(2).bitcast(BF16)

    def proj_tile(ti, jj):
        T = "p"
        norm_transpose_tile(c, x_in[ti * 128:(ti + 1) * 128, :], xt, junk, ssq, rstd, xn, gb, xnT[:], 2, T)
        for k in range(8):
            c.P(lambda e, k=k: e.matmul(c.bank(0), lhsT=xnT[:, k, :], rhs=wb[:, k, 0:512], start=(k == 0), stop=(k == 7)),
                r=[(T, "xnT"), "wb"], w=[("bk", 0)])
        for k in range(8):
            c.P(lambda e, k=k: e.matmul(c.bank(1)[:, 0:320], lhsT=xnT[:, k, :], rhs=wb[:, k, 512:832], start=(k == 0), stop=(k == 7)),
                r=[(T, "xnT"), "wb"], w=[("bk", 1)])
        z0, z1 = c.bank(0), c.bank(1)
        if stage == 1 and SUB[0] <= 1:
            return z0, z1
        rms_rows(c, z0[:, 0:256], 256, g_cq, cqn[:], junk[:, 0:256], st[:, 0:1], [("bk", 0)], "cq", "cqn")
        for kc in range(2):
            c.P(lambda e, kc=kc: e.transpose(out=pTb[:, kc * 128:(kc + 1) * 128], in_=cqn[:, kc * 128:(kc + 1) * 128], identity=c.identb[:]),
                r=["cqn", "identb"], w=[("bk", 2)])
        c.V(lambda e: e.tensor_copy(out=cqT[:].rearrange("p a t -> p (a t)"), in_=pTb[:, 0:256]), r=[("bk", 2)], w=["cqT"])
        for kc in range(2):
            c.P(lambda e, kc=kc: e.matmul(c.bank(3)[:, 0:192], lhsT=cqT[:, kc, :], rhs=wuq[:, kc, :], start=(kc == 0), stop=(kc == 1)),
                r=["cqT", "wuq"], w=[("bk", 3)])
        rms_rows(c, c.bank(3)[:, 0:192], 192, g_q, qk[:, 0, :], junk[:, 0:192], st[:, 1:2], [("bk", 3)], "qc", ("qk", 0))
        if stage == 1 and SUB[0] <= 2:
            return z0, z1
        rms_rows(c, z0[:, 256:384], 128, g_ckv, ckvn[:], junk[:, 0:128], st[:, 2:3], [("bk", 0)], "ckv", "ckvn")
        c.P(lambda e: e.transpose(out=pTb[:, 256:384], in_=ckvn[:], identity=c.identb[:]), r=["ckvn", "identb"], w=[("bk", 2)])
        c.V(lambda e: e.tensor_copy(out=ckvT[:], in_=pTb[:, 256:384]), r=[("bk", 2)], w=["ckvT"])
        c.P(lambda e: e.matmul(c.bank(3)[:, 256:512], lhsT=ckvT[:], rhs=wukv[:], start=True, stop=True), r=["ckvT", "wukv"], w=[("bk", 3)])
        c.A(lambda e: e.copy(out=kcat[:, 0:128], in_=c.bank(3)[:, 256:384]), r=[("bk", 3)], w=["kcat"])
        c.A(lambda e: e.copy(out=kcat[:, 128:192], in_=z0[:, 384:448]), r=[("bk", 0)], w=["kcat"])
        c.A(lambda e: e.copy(out=Vc[:, ti, 0:128], in_=c.bank(3)[:, 384:512]), r=[("bk", 3)], w=["Vc"])
        rms_rows(c, kcat[:], 192, g_k, qk[:, 1, :], junk[:, 0:192], st[:, 3:4], ["kcat"], "kc", ("qk", 1))
        if stage == 1 and SUB[0] <= 3:
            return z0, z1
        x1 = qk[:, :, 128:160]
        x2 = qk[:, :, 160:192]
        cs = cosT[:, ti, :].unsqueeze(1).to_broadcast([128, 2, 32])
        sn = sinT[:, ti, :].unsqueeze(1).to_broadcast([128, 2, 32])
        rk = [("qk", 0), ("qk", 1), "sinT", "cosT"]
        c.V(lambda e: e.tensor_tensor(out=rt[:, 0], in0=x1, in1=cs, op=ALU.mult), r=rk, w=["rt0"])
        c.V(lambda e: e.tensor_tensor(out=rt[:, 1], in0=x2, in1=sn, op=ALU.mult), r=rk, w=["rt1"])
        c.V(lambda e: e.tensor_tensor(out=rt[:, 2], in0=x2, in1=cs, op=ALU.mult), r=rk, w=["rt2"])
        c.V(lambda e: e.tensor_tensor(out=rt[:, 3], in0=x1, in1=sn, op=ALU.mult), r=rk, w=["rt3"])
        c.V(lambda e: e.tensor_copy(out=qkb[:, :, 0:128], in_=qk[:, :, 0:128]), r=[("qk", 0), ("qk", 1)], w=["qkb0"])
        c.V(lambda e: e.tensor_tensor(out=qkb[:, :, 128:160], in0=rt[:, 0], in1=rt[:, 1], op=ALU.subtract), r=["rt0", "rt1"], w=["qkb1"])
        c.V(lambda e: e.tensor_tensor(out=qkb[:, :, 160:192], in0=rt[:, 2], in1=rt[:, 3], op=ALU.add), r=["rt2", "rt3"], w=["qkb2"])
        if stage == 1 and SUB[0] <= 4:
            return z0, z1
        qkk = ["qkb0", "qkb1", "qkb2", "identb"]
        c.P(lambda e: e.transpose(out=pTb[:, 0:128], in_=qkb[:, 0, 0:128], identity=c.identb[:]), r=qkk, w=[("bk", 2)])
        c.P(lambda e: e.transpose(out=pTb[:, 128:256], in_=qkb[:, 0, 128:256], identity=c.identb[:]), r=qkk, w=[("bk", 2)])
        c.P(lambda e: e.transpose(out=pTb[:, 256:384], in_=qkb[:, 1, 0:128], identity=c.identb[:]), r=qkk, w=[("bk", 2)])
        c.P(lambda e: e.transpose(out=pTb[:, 384:512], in_=qkb[:, 1, 128:256], identity=c.identb[:]), r=qkk, w=[("bk", 2)])
        sl = slice(jj * 128, (jj + 1) * 128)
        gs = slice(ti * 128, (ti + 1) * 128)
        if stage == 1 and SUB[0] <= 5:
            return z0, z1
        c.V(lambda e: e.tensor_copy(out=qa[:, sl], in_=pTb[:, 0:128]), r=[("bk", 2)], w=["qa"])
        if stage == 1 and SUB[0] <= 6:
            return z0, z1
        c.V(lambda e: e.tensor_copy(out=qb[:, sl], in_=pTb[:, 128:256]), r=[("bk", 2)], w=["qb"])
        if stage == 1 and SUB[0] <= 7:
            return z0, z1
        c.V(lambda e: e.tensor_copy(out=kcTa[:, gs], in_=pTb[:, 256:384]), r=[("bk", 2)], w=["kcTa"])
        c.V(lambda e: e.tensor_copy(out=kcTb[:, gs], in_=pTb[:, 384:512]), r=[("bk", 2)], w=["kcTb"])
        return z0, z1

    def moba_tile(ti, jj, z0, z1):
        sl = slice(jj * 128, (jj + 1) * 128)
        gs = slice(ti * 128, (ti + 1) * 128)
        jb = ti // 2
        c.A(lambda e: e.copy(out=qdn[:, 0:64], in_=z0[:, 448:512]), r=[("bk", 0)], w=["qdn"])
        c.A(lambda e: e.copy(out=qdn[:, 64:128], in_=z1[:, 0:64]), r=[("bk", 1)], w=["qdn"])
        rms_rows(c, qdn[:], 128, g_mq, qdn[:], junk[:, 0:128], st[:, 4:5], ["qdn"], "mq", "qdn")
        rms_rows(c, z1[:, 64:192], 128, g_mk, kdn[:], junk[:, 0:128], st[:, 5:6], [("bk", 1)], "mk", "kdn")
        c.A(lambda e: e.copy(out=Vd[:, ti, 0:128], in_=z1[:, 192:320]), r=[("bk", 1)], w=["Vd"])
        c.V(lambda e: e.tensor_copy(out=qdb[:], in_=qdn[:]), r=["qdn"], w=["qdb"])
        c.V(lambda e: e.tensor_copy(out=kdb[:], in_=kdn[:]), r=["kdn"], w=["kdb"])
        c.P(lambda e: e.transpose(out=pTb[:, 512:640], in_=qdb[:], identity=c.identb[:]), r=["qdb", "identb"], w=[("bk", 2)])
        c.P(lambda e: e.transpose(out=pTb[:, 640:768], in_=kdb[:], identity=c.identb[:]), r=["kdb", "identb"], w=[("bk", 2)])
        c.V(lambda e: e.tensor_copy(out=qd[:, sl], in_=pTb[:, 512:640]), r=[("bk", 2)], w=["qd"])
        c.V(lambda e: e.tensor_copy(out=kdT[:, gs], in_=pTb[:, 640:768]), r=[("bk", 2)], w=["kdT"])
        c.P(lambda e: e.transpose(out=c.bank(3)[:, 0:128], in_=qdn[:], identity=c.identf[:]), r=["qdn", "identf"], w=[("bk", 3)])
        c.V(lambda e: e.tensor_copy(out=qdTf[:], in_=c.bank(3)[:, 0:128]), r=[("bk", 3)], w=["qdTf"])
        c.P(lambda e: e.matmul(c.bank(3)[:, 128:160], lhsT=qdTf[:], rhs=kmT[:], start=True, stop=True), r=["qdTf", "kmT"], w=[("bk", 3)])
        c.V(lambda e: e.tensor_copy(out=gsb[:], in_=c.bank(3)[:, 128:160]), r=[("bk", 3)], w=["gsb"])
        c.V(lambda e: e.memset(gsb[:, jb:32], -BIG), r=["gsb"], w=["gsb"])
        c.V(lambda e: e.max(out=m8[:], in_=gsb[:]), r=["gsb"], w=["m8"])
        c.V(lambda e: e.memset(self_[:], 0.0), w=["self"])
        if jb > 0:
            c.V(lambda e: e.tensor_scalar(out=self_[:, 0:jb], in0=gsb[:, 0:jb], scalar1=m8[:, 2:3], scalar2=None, op0=ALU.is_ge),
                r=["gsb", "m8", "self"], w=["self"])
        c.V(lambda e: e.memset(self_[:, jb:jb + 1], 1.0), r=["self"], w=["self"])
        c.V(lambda e: e.tensor_scalar(out=Mb[:, 0:32], in0=self_[:], scalar1=-1.0, scalar2=BIG, op0=ALU.add, op1=ALU.mult), r=["self", "Mb"], w=["Mb"])
        c.P(lambda e: e.transpose(out=pTb[:, 768:896], in_=Mb[:], identity=c.identb[:]), r=["Mb", "identb"], w=[("bk", 2)])
        c.V(lambda e: e.tensor_copy(out=MT[:, sl], in_=pTb[:, 768:896]), r=[("bk", 2)], w=["MT"])
        half = ti % 2
        c.P(lambda e: e.matmul(c.bank(3)[:, 192:193], lhsT=kdn[:], rhs=ones256[:], start=True, stop=True),
            r=["kdn", "ones256"], w=[("bk", 3)])
        if half == 0:
            c.V(lambda e: e.tensor_copy(out=ktmp[:], in_=c.bank(3)[:, 192:193]), r=[("bk", 3)], w=["ktmp"])
        else:
            c.V(lambda e: e.tensor_tensor(out=kmT[:, jb:jb + 1], in0=ktmp[:], in1=c.bank(3)[:, 192:193], op=ALU.add),
                r=[("bk", 3), "ktmp"], w=["kmT"])

    def attention(j, which):
        scale = (192 ** -0.5) if which == 0 else (128 ** -0.5)
        nk = 4 * j + 4
        Vt = Vc if which == 0 else Vd
        vk = "Vc" if which == 0 else "Vd"

        def score(i):
            sb_ = i % 2
            ks = slice(i * 128, (i + 1) * 128)
            if which == 0:
                c.P(lambda e: e.matmul(c.bank(sb_), lhsT=kcTa[:, ks], rhs=qa[:], start=True, stop=False),
                    r=["kcTa", "qa"], w=[("bk", sb_)])
                c.P(lambda e: e.matmul(c.bank(sb_), lhsT=kcTb[:, ks], rhs=qb[:], start=False, stop=True),
                    r=["kcTb", "qb"], w=[("bk", sb_)])
            else:
                c.P(lambda e: e.matmul(c.bank(sb_), lhsT=kdT[:, ks], rhs=qd[:], start=True, stop=False),
                    r=["kdT", "qd"], w=[("bk", sb_)])
                c.P(lambda e: e.matmul(c.bank(sb_), lhsT=Eblk[:, i // 2, :], rhs=MT[:], start=False, stop=True),
                    r=["Eblk", "MT"], w=[("bk", sb_)])

        score(0)
        for i in range(nk):
            sb_ = i % 2
            pp = i % 2
            if i + 1 < nk:
                score(i + 1)
            c.A(lambda e, pp=pp, sb_=sb_: e.activation(out=PT[pp][:], in_=c.bank(sb_), func=AF.Exp, scale=float(scale)),
                r=[("bk", sb_)], w=[("PT", pp)])
            m = i - 4 * j
            if m >= 0:
                c.V(lambda e, pp=pp, m=m: e.tensor_tensor(out=PT[pp][:], in0=PT[pp][:], in1=maskD[:, m, :], op=ALU.mult),
                    r=[("PT", pp), "maskD"], w=[("PT", pp)])
            for jj in range(4):
                if i > 4 * j + jj:
                    continue
                c.P(lambda e, pp=pp, jj=jj, i=i: e.matmul(c.bank(4 + jj)[:, 0:129], lhsT=PT[pp][:, jj * 128:(jj + 1) * 128], rhs=Vt[:, i, :],
                                                          start=(i == 0), stop=(i == 4 * j + jj)),
                    r=[("PT", pp), vk], w=[("bk", 4 + jj)])
        for jj in range(4):
            acc = c.bank(4 + jj)
            c.V(lambda e, acc=acc: e.reciprocal(out=rec[:], in_=acc[:, 128:129]), r=[("bk", 4 + jj)], w=["rec"])
            c.V(lambda e, acc=acc, jj=jj: e.tensor_scalar(out=otile[jj][:, which * 128:(which + 1) * 128], in0=acc[:, 0:128],
                                                          scalar1=rec[:, 0:1], scalar2=None, op0=ALU.mult),
                r=[("bk", 4 + jj), "rec"], w=[("ot", jj, which)])

    if stage < 9:
        for jj in range(4):
            c.V(lambda e, jj=jj: e.memset(otile[jj][:], 0.0), w=[("ot", jj, 0), ("ot", jj, 1)])
    for j in range(NQB):
        for jj in range(4):
            ti = 4 * j + jj
            if stage >= 1:
                z0, z1 = proj_tile(ti, jj)
            if stage >= 2:
                moba_tile(ti, jj, z0, z1)
        if stage >= 3:
            attention(j, 0)
        if stage >= 4:
            attention(j, 1)
        for jj in range(4):
            ti = 4 * j + jj
            c.S.dma("sp", lambda e, s, jj=jj, ti=ti: e.dma_start(out=out[ti * 128:(ti + 1) * 128, :], in_=otile[jj][:]).then_inc(s, 16),
                    reads=[("ot", jj, 0), ("ot", jj, 1)], writes=[("out", ti)], stream=("out", jj))
    return c.finish([("out", ti) for ti in range(NTL)])


def l1_inputs(xb, pos_b, norm_mix, w_in, cq_norm, ckv_norm, w_uq, w_ukv, mla_q_norm, mla_k_norm, moba_q_norm, moba_k_norm, h):
    cols = np.concatenate([np.arange(0, 448), 448 + h * 128 + np.arange(128), 960 + h * 128 + np.arange(128),
                           1472 + h * 128 + np.arange(128)])
    gains = np.concatenate([cq_norm, ckv_norm, mla_q_norm, mla_k_norm, moba_q_norm, moba_k_norm]).reshape(1, 1024)
    invf = (10000.0 ** (-np.arange(32, dtype=np.float32) / 32)).astype(np.float32).reshape(1, 32)
    return {"x": np.ascontiguousarray(xb), "gmix": np.ascontiguousarray(norm_mix.reshape(1, 1024)),
            "w832": np.ascontiguousarray(w_in[:, cols]), "gains": np.ascontiguousarray(gains.astype(np.float32)),
            "wuq": np.ascontiguousarray(w_uq[:, h * 192:(h + 1) * 192]),
            "wukv": np.ascontiguousarray(w_ukv[:, h * 256:(h + 1) * 256]),
            "pos": np.ascontiguousarray(pos_b.astype(np.int32).reshape(-1, 128).T), "invf": invf}


_PROGS = {}


def _prog(name, fn, *a):
    k = (name,) + a
    if k not in _PROGS:
        _PROGS[k] = fn(*a)
    return _PROGS[k]


def _run(nc, maps):
    return run_bass_kernel_spmd(nc, maps, core_ids=list(range(len(maps)))).results


def kernel(**inp):
    inp = {k: np.asarray(v) for k, v in inp.items()}
    x = inp["x"].astype(np.float32)
    B, S, D = x.shape
    NTOK = B * S
    NC = 8
    per = NTOK // NC

    def assemble(res):
        mix = np.empty((B, S, D), np.float32)
        for b in range(B):
            for h in range(4):
                o = res[b * 4 + h]["out"]
                mix[b, :, h * 128:(h + 1) * 128] = o[:, :128]
                mix[b, :, 512 + h * 128:512 + (h + 1) * 128] = o[:, 128:]
        return mix

    def token_phase(xcur, mix, L):
        nct = _prog("tok", build_token_phase, per)
        xf = xcur.reshape(NTOK, D)
        mf = mix.reshape(NTOK, D)
        base = token_inputs(xf[0:per], mf[0:per], inp[L + "_w_out"], inp[L + "_norm_ffn"], inp[L + "_peer_w_query"],
                            inp[L + "_peer_sub_keys"], inp[L + "_peer_expert_down"], inp[L + "_peer_expert_up"])
        maps = []
        for ci in range(NC):
            m = dict(base)
            m["x"] = np.ascontiguousarray(xf[ci * per:(ci + 1) * per])
            m["mix"] = np.ascontiguousarray(mf[ci * per:(ci + 1) * per])
            maps.append(m)
        res = _run(nct, maps)
        return np.concatenate([r["out"] for r in res], axis=0).reshape(B, S, D)

    nc0 = _prog("l0", build_l0_mixer, S)
    maps = [l0_inputs(x[b], inp["l0_norm_mix"], inp["l0_w_in"], inp["lb_logits"], inp["l0_hgrn_out_norm"], inp["l0_gmlp_v_norm"],
                      inp["l0_gmlp_w_s"], inp["l0_gmlp_b_s"], h) for b in range(B) for h in range(4)]
    mix = assemble(_run(nc0, maps))
    x = token_phase(x, mix, "l0")
    nc1 = _prog("l1", build_l1_mixer, S)
    maps = [l1_inputs(x[b], inp["positions"][b], inp["l1_norm_mix"], inp["l1_w_in"], inp["l1_mla_cq_norm"], inp["l1_mla_ckv_norm"],
                      inp["l1_mla_w_uq"], inp["l1_mla_w_ukv"], inp["l1_mla_q_norm"], inp["l1_mla_k_norm"], inp["l1_moba_q_norm"],
                      inp["l1_moba_k_norm"], h) for b in range(B) for h in range(4)]
    mix = assemble(_run(nc1, maps))
    x = token_phase(x, mix, "l1")
    return x.astype(np.float32)
```

```python
import numpy as np
from contextlib import ExitStack
import concourse.bass as bass
import concourse.mybir as mybir
from concourse.bass_utils import run_bass_kernel_spmd

F32 = mybir.dt.float32
BF16 = mybir.dt.bfloat16
I32 = mybir.dt.int32
U32 = mybir.dt.uint32
AF = mybir.ActivationFunctionType
ALU = mybir.AluOpType
AX = mybir.AxisListType

ENGS = ("pe", "act", "dve", "pool", "sp")
RMS_EPS = 1e-6


class _Op:
    __slots__ = ("eng", "fn", "reads", "writes", "dma", "stream", "deps", "signal",
                 "sigval", "ndma", "idx")

    def __init__(self, eng, fn, reads, writes, dma, stream):
        self.eng = eng
        self.fn = fn
        self.reads = reads
        self.writes = writes
        self.dma = dma
        self.stream = stream
        self.deps = {}
        self.signal = False
        self.sigval = None
        self.ndma = 0


class Sched:
    def __init__(self, nc):
        self.nc = nc
        self.ops = []

    def op(self, eng, fn, reads=(), writes=()):
        self.ops.append(_Op(eng, fn, tuple(reads), tuple(writes), False, None))

    def dma(self, eng, fn, reads=(), writes=(), stream=None, n=1):
        o = _Op(eng, fn, tuple(reads), tuple(writes), True, stream)
        o.ndma = n
        self.ops.append(o)

    def wait_all(self, eng, keys):
        self.ops.append(_Op(eng, None, tuple(keys), (), False, None))

    def emit(self, stack):
        nc = self.nc
        ops = self.ops
        writers = {}
        readers = {}
        last_on_stream = {}
        for i, op in enumerate(ops):
            op.idx = i
            deps = {}
            for k in op.reads:
                for w in writers.get(k, ()):
                    deps[w] = True
            for k in op.writes:
                for w in writers.get(k, ()):
                    if w not in deps:
                        deps[w] = False
                for r in readers.get(k, ()):
                    if r not in deps:
                        deps[r] = False
            if op.dma:
                p = last_on_stream.get(op.stream)
                if p is not None and p not in deps:
                    deps[p] = True
                last_on_stream[op.stream] = i
            for k in op.reads:
                readers.setdefault(k, []).append(i)
            for k in op.writes:
                if readers.get(k):
                    writers[k] = [i]
                    readers[k] = []
                else:
                    writers.setdefault(k, []).append(i)
            deps.pop(i, None)
            fd = {}
            for d, raw in deps.items():
                p = ops[d]
                if p.fn is None:
                    continue
                if (not p.dma) and p.eng == op.eng and p.eng == "pe":
                    continue
                fd[d] = raw
                p.signal = True
            op.deps = fd
        cnt = {e: 0 for e in ENGS}
        scnt = {}
        for op in ops:
            if op.fn is None:
                continue
            if op.dma:
                scnt[op.stream] = scnt.get(op.stream, 0) + 16 * op.ndma
                op.sigval = scnt[op.stream]
            elif op.signal:
                cnt[op.eng] += 1
                op.sigval = cnt[op.eng]
        esem = {e: stack.enter_context(nc.semaphore("sem_" + e)) for e in ENGS}
        ssem = {}
        for s in scnt:
            ssem[s] = stack.enter_context(nc.semaphore("dsem_%d" % len(ssem)))
        self.n_sems = len(esem) + len(ssem)
        self.counts = dict(cnt)
        block = stack.enter_context(nc.Block())
        byeng = {e: [o for o in ops if o.eng == e] for e in ENGS}

        def run(eng_name, engine):
            waited = {}
            for op in byeng[eng_name]:
                need = {}
                for d in op.deps:
                    p = ops[d]
                    if p.dma:
                        key = ("s", p.stream)
                        sem = ssem[p.stream]
                    else:
                        key = ("e", p.eng)
                        sem = esem[p.eng]
                    if waited.get(key, 0) >= p.sigval:
                        continue
                    if need.get(key, (None, 0))[1] < p.sigval:
                        need[key] = (sem, p.sigval)
                for key, (sem, val) in need.items():
                    engine.wait_ge(sem, val)
                    waited[key] = val
                if op.fn is None:
                    continue
                if op.dma:
                    op.fn(engine, ssem[op.stream])
                else:
                    ins = op.fn(engine)
                    if op.signal:
                        if isinstance(ins, (list, tuple)):
                            ins = ins[-1]
                        ins.then_inc(esem[op.eng], 1)

        block.tensor(lambda e: run("pe", e))
        block.scalar(lambda e: run("act", e))
        block.vector(lambda e: run("dve", e))
        block.gpsimd(lambda e: run("pool", e))
        block.sync(lambda e: run("sp", e))


class Ctx:
    def __init__(self):
        self.nc = bass.Bass("TRN2", target_bir_lowering=False)
        self.st = ExitStack()
        self.S = Sched(self.nc)
        self._n = 0
        self.PS = self.st.enter_context(self.nc.psum_tensor("PS", [128, 4096], F32))
        self.outs = []

    def din(self, name, shape, dt=F32):
        return self.nc.dram_tensor(name, list(shape), dt, kind="ExternalInput").ap()

    def dout(self, name, shape, dt=F32):
        self.outs.append(name)
        return self.nc.dram_tensor(name, list(shape), dt, kind="ExternalOutput").ap()

    def sb(self, shape, dt=F32, name=None):
        self._n += 1
        return self.st.enter_context(self.nc.sbuf_tensor("s_" + (name or ("t%d" % self._n)), list(shape), dt))

    def bank(self, i, n=1):
        return self.PS[:, i * 512:(i + n) * 512]

    def V(self, fn, r=(), w=()):
        self.S.op("dve", fn, r, w)

    def A(self, fn, r=(), w=()):
        self.S.op("act", fn, r, w)

    def P(self, fn, r=(), w=()):
        self.S.op("pe", fn, r, w)

    def G(self, fn, r=(), w=()):
        self.S.op("pool", fn, r, w)

    def load(self, eng, out, in_, key, stream=None, reads=()):
        self.S.dma(eng, lambda e, s: e.dma_start(out=out, in_=in_).then_inc(s, 16),
                   reads=list(reads), writes=[key], stream=stream or key)

    def store(self, eng, out, in_, rkey, okey, stream=None):
        self.S.dma(eng, lambda e, s: e.dma_start(out=out, in_=in_).then_inc(s, 16),
                   reads=[rkey], writes=[okey], stream=stream or okey)

    def finish(self, final_keys):
        self.S.wait_all("sp", final_keys)
        self.S.emit(self.st)
        self.st.close()
        return self.nc

    def consts(self):
        io = self.sb([128, 128], I32)
        io2 = self.sb([128, 128], I32)
        self.identb = self.sb([128, 128], BF16)
        self.identf = self.sb([128, 128], F32)
        self.iotaF = self.sb([128, 128], F32)
        self.G(lambda e: e.iota(io[:], pattern=[[1, 128]], base=0, channel_multiplier=-1), w=["io"])
        self.G(lambda e: e.iota(io2[:], pattern=[[1, 128]], base=0, channel_multiplier=0), w=["io2"])
        self.V(lambda e: e.tensor_scalar(out=self.identb[:], in0=io[:], scalar1=0.0, scalar2=None, op0=ALU.is_equal),
               r=["io"], w=["identb"])
        self.V(lambda e: e.tensor_scalar(out=self.identf[:], in0=io[:], scalar1=0.0, scalar2=None, op0=ALU.is_equal),
               r=["io"], w=["identf"])
        self.V(lambda e: e.tensor_copy(out=self.iotaF[:], in_=io2[:]), r=["io2"], w=["iotaF"])


TB = 256
NW = 10
LA = 3


def build_token_phase(NT, n_i1=128):
    c = Ctx()
    nc = c.nc
    x_in = c.din("x", [NT, 1024])
    mix = c.din("mix", [NT, 1024])
    w_out = c.din("w_out", [1024, 1024])
    gffn = c.din("gffn", [1, 1024])
    wq = c.din("wq", [16, 128, 1024])
    skT = c.din("skT", [128, 16 * 128])
    dT = c.din("dT", [128, 128, 1024])
    up = c.din("up", [16384, 1024])
    out = c.dout("out", [NT, 1024])
    c.consts()
    NB = NT // TB

    gb = c.sb([128, 1024], F32)
    c.load("sp", gb[:], gffn.to_broadcast([128, 1024]), "gb")
    skb = c.sb([128, 16 * 128], BF16)
    c.load("pool", skb[:], skT, "skb")
    iota16 = c.iotaF[:, 0:16]

    wbuf = [c.sb([128, 1024], BF16, name="wbuf%d" % i) for i in range(NW)]
    wctr = [0]

    def wload(src, eng="pool", reads=()):
        s = wctr[0] % NW
        wctr[0] += 1
        c.load(eng, wbuf[s][:], src, ("w", s), reads=reads)
        return s

    dTb = nc.dram_tensor("dTb", [128, 128, 1024], BF16, kind="Internal").ap()
    upb = nc.dram_tensor("upb", [16384, 1024], BF16, kind="Internal").ap()
    for i1 in range(n_i1):
        c.S.dma("pool", lambda e, s, i1=i1: e.dma_start(out=dTb[i1], in_=dT[i1]).then_inc(s, 16),
                writes=[("dTb", i1)], stream=("pc", (2 * i1) % 8))
        c.S.dma("pool", lambda e, s, i1=i1: e.dma_start(out=upb[i1 * 128:(i1 + 1) * 128, :], in_=up[i1 * 128:(i1 + 1) * 128, :]).then_inc(s, 16),
                writes=[("upb", i1)], stream=("pc", (2 * i1 + 1) % 8))

    xt = [c.sb([128, 1024], F32, name="xt%d" % j) for j in range(2)]
    mt = c.sb([128, 1024], F32, name="mt")
    mb = c.sb([128, 1024], BF16, name="mb")
    mixT = [c.sb([128, 8, 128], BF16, name="mixT%d" % j) for j in range(2)]
    x1 = [c.sb([128, 1024], F32, name="x1_%d" % j) for j in range(2)]
    junk = c.sb([128, 1024], F32, name="junk")
    ssq = c.sb([128, 2], F32, name="ssq")
    rstd = c.sb([128, 2], F32, name="rstd")
    xn = c.sb([128, 1024], BF16, name="xn")
    xnT = c.sb([128, 8, TB], BF16, name="xnT")
    qT = c.sb([128, 16, TB], BF16, name="qT")
    bufA = c.sb([128, 2048], F32, name="bufA")
    bufB = c.sb([128, 2048], F32, name="bufB")
    v = c.sb([128, 16, 16], F32, name="v")
    ix = c.sb([128, 16, 16], U32, name="ix")
    ixf = c.sb([128, 16, 16], F32, name="ixf")
    ts_ = c.sb([128, 8, 16], F32, name="ts")
    tc_ = c.sb([128, 8, 16], U32, name="tc")
    ai = c.sb([128, 8, 16], U32, name="ai")
    bi = c.sb([128, 8, 16], U32, name="bi")
    af = c.sb([128, 8, 16], F32, name="af")
    bf = c.sb([128, 8, 16], F32, name="bf")
    IG = c.sb([128, 3, 128], F32, name="IG")
    dd = c.sb([128, 8, 16], F32, name="dd")
    ee = c.sb([128, 8, 16], F32, name="ee")
    es = c.sb([128, 8], F32, name="es")
    rs = c.sb([128, 8], F32, name="rs")
    IT = c.sb([128, 3, TB], F32, name="IT")
    NS = 4
    At = [c.sb([128, 128], BF16, name="At%d" % i) for i in range(NS)]
    Bt = [c.sb([128, 128], BF16, name="Bt%d" % i) for i in range(NS)]
    GT = c.sb([128, 128, TB], BF16, name="GT")
    sq = [c.sb([128, TB], F32, name="sq%d" % i) for i in range(4)]
    uu = [c.sb([128, TB], F32, name="uu%d" % i) for i in range(4)]
    u2 = [c.sb([128, TB], F32, name="u2%d" % i) for i in range(4)]
    sg = [c.sb([128, TB], F32, name="sg%d" % i) for i in range(4)]
    hg = [c.sb([128, TB], F32, name="hg%d" % i) for i in range(4)]
    AT = [c.sb([128, TB], BF16, name="AT%d" % i) for i in range(4)]
    xo = [c.sb([128, 1024], F32, name="xo%d" % j) for j in range(2)]

    pTb = c.bank(4).bitcast(BF16).rearrange("p (k t) -> p k t", k=8)
    pTf = c.bank(4)[:, 0:384].rearrange("p (k t) -> p k t", k=3)

    for b in range(NB):
        for j in range(2):
            r0 = b * TB + j * 128
            c.load("sp", xt[j][:], x_in[r0:r0 + 128, :], ("xt", j))
            c.load("sp", mt[:], mix[r0:r0 + 128, :], "mt")
            c.A(lambda e: e.copy(out=mb[:], in_=mt[:]), r=["mt"], w=["mb"])
            for k in range(8):
                c.P(lambda e, k=k: e.transpose(out=pTb[:, k, :], in_=mb[:, k * 128:(k + 1) * 128], identity=c.identb[:]),
                    r=["mb", "identb"], w=[("bk", 4)])
            c.V(lambda e, j=j: e.tensor_copy(out=mixT[j][:], in_=pTb), r=[("bk", 4)], w=[("mixT", j)])
        for k in range(8):
            s = wload(w_out[k * 128:(k + 1) * 128, :])
            for j in range(2):
                for hf in range(2):
                    c.P(lambda e, k=k, j=j, hf=hf, s=s: e.matmul(
                        c.bank(j * 2 + hf), lhsT=mixT[j][:, k, :], rhs=wbuf[s][:, hf * 512:(hf + 1) * 512],
                        start=(k == 0), stop=(k == 7)),
                        r=[("mixT", j), ("w", s)], w=[("bk", j * 2 + hf)])
        for j in range(2):
            c.V(lambda e, j=j: e.tensor_tensor(out=x1[j][:], in0=xt[j][:], in1=c.bank(j * 2, 2), op=ALU.add),
                r=[("xt", j), ("bk", j * 2), ("bk", j * 2 + 1)], w=[("x1", j)])
        for j in range(2):
            c.A(lambda e, j=j: e.activation(out=junk[:], in_=x1[j][:], func=AF.Square, accum_out=ssq[:, j:j + 1]),
                r=[("x1", j)], w=["junk", ("ssq", j)])
            c.V(lambda e, j=j: e.tensor_scalar(out=rstd[:, j:j + 1], in0=ssq[:, j:j + 1], scalar1=1.0 / 1024, scalar2=RMS_EPS,
                                               op0=ALU.mult, op1=ALU.add), r=[("ssq", j)], w=[("rstd", j)])
            c.A(lambda e, j=j: e.activation(out=rstd[:, j:j + 1], in_=rstd[:, j:j + 1], func=AF.Sqrt),
                r=[("rstd", j)], w=[("rstd", j)])
            c.V(lambda e, j=j: e.reciprocal(out=rstd[:, j:j + 1], in_=rstd[:, j:j + 1]), r=[("rstd", j)], w=[("rstd", j)])
            c.V(lambda e, j=j: e.scalar_tensor_tensor(out=xn[:], in0=x1[j][:], scalar=rstd[:, j:j + 1], in1=gb[:],
                                                      op0=ALU.mult, op1=ALU.mult),
                r=[("x1", j), ("rstd", j), "gb"], w=["xn"])
            for k in range(8):
                c.P(lambda e, k=k: e.transpose(out=pTb[:, k, :], in_=xn[:, k * 128:(k + 1) * 128], identity=c.identb[:]),
                    r=["xn", "identb"], w=[("bk", 4)])
            c.V(lambda e, j=j: e.tensor_copy(out=xnT[:, :, j * 128:(j + 1) * 128], in_=pTb), r=[("bk", 4)], w=["xnT"])
        for cc in range(16):
            s = wload(wq[cc])
            hb = 5 + (cc % 2)
            for k in range(8):
                c.P(lambda e, k=k, s=s, hb=hb: e.matmul(c.bank(hb)[:, 0:TB], lhsT=wbuf[s][:, k * 128:(k + 1) * 128],
                                                        rhs=xnT[:, k, :], start=(k == 0), stop=(k == 7)),
                    r=[("w", s), "xnT"], w=[("bk", hb)])
            if cc % 2 == 0:
                c.A(lambda e, cc=cc, hb=hb: e.copy(out=qT[:, cc, :], in_=c.bank(hb)[:, 0:TB]), r=[("bk", hb)], w=[("qT", cc)])
            else:
                c.V(lambda e, cc=cc, hb=hb: e.tensor_copy(out=qT[:, cc, :], in_=c.bank(hb)[:, 0:TB]), r=[("bk", hb)], w=[("qT", cc)])
        for j in range(2):
            for cc in range(16):
                c.P(lambda e, cc=cc, j=j: e.matmul(c.bank(0, 4)[:, cc * 128:(cc + 1) * 128], lhsT=qT[:, cc, j * 128:(j + 1) * 128],
                                                   rhs=skb[:, cc * 128:(cc + 1) * 128], start=True, stop=True),
                    r=[("qT", cc), "skb"], w=[("bk", cc // 4)])
            c.V(lambda e, j=j: e.tensor_copy(out=bufA[:], in_=c.bank(0, 4)),
                r=[("bk", 0), ("bk", 1), ("bk", 2), ("bk", 3)], w=["bufA"])
            A3 = bufA[:].rearrange("p (c n) -> p c n", c=16)
            B3 = bufB[:].rearrange("p (c n) -> p c n", c=16)
            for cc in range(16):
                c.V(lambda e, cc=cc: e.max(out=v[:, cc, 0:8], in_=A3[:, cc, :]), r=["bufA"], w=[("v", cc)])
                c.V(lambda e, cc=cc: e.max_index(out=ix[:, cc, 0:8], in_max=v[:, cc, 0:8], in_values=A3[:, cc, :]),
                    r=["bufA", ("v", cc)], w=[("ix", cc)])
                c.V(lambda e, cc=cc: e.match_replace(out=B3[:, cc, :], in_to_replace=v[:, cc, 0:8], in_values=A3[:, cc, :], imm_value=-1e30),
                    r=["bufA", ("v", cc)], w=[("bufB", cc)])
                c.V(lambda e, cc=cc: e.max(out=v[:, cc, 8:16], in_=B3[:, cc, :]), r=[("bufB", cc)], w=[("v2", cc)])
                c.V(lambda e, cc=cc: e.max_index(out=ix[:, cc, 8:16], in_max=v[:, cc, 8:16], in_values=B3[:, cc, :]),
                    r=[("bufB", cc), ("v2", cc)], w=[("ix2", cc)])
            vall = [("v", cc) for cc in range(16)] + [("v2", cc) for cc in range(16)]
            ixall = [("ix", cc) for cc in range(16)] + [("ix2", cc) for cc in range(16)]
            c.V(lambda e: e.tensor_copy(out=ixf[:], in_=ix[:]), r=ixall, w=["ixf"])
            v4 = v[:].rearrange("p (h two) a -> p h two a", two=2)
            ixf4 = ixf[:].rearrange("p (h two) a -> p h two a", two=2)
            cand = bufA[:].rearrange("p (h a b) -> p h a b", h=8, a=16)
            candf = bufA[:].rearrange("p (h n) -> p h n", h=8)
            candB = bufB[:].rearrange("p (h n) -> p h n", h=8)
            c.V(lambda e: e.tensor_tensor(out=cand, in0=v4[:, :, 0, :].unsqueeze(3).to_broadcast([128, 8, 16, 16]),
                                          in1=v4[:, :, 1, :].unsqueeze(2).to_broadcast([128, 8, 16, 16]), op=ALU.add),
                r=vall, w=["bufA"])
            for h in range(8):
                c.V(lambda e, h=h: e.max(out=ts_[:, h, 0:8], in_=candf[:, h, :]), r=["bufA"], w=[("ts", h)])
                c.V(lambda e, h=h: e.max_index(out=tc_[:, h, 0:8], in_max=ts_[:, h, 0:8], in_values=candf[:, h, :]),
                    r=["bufA", ("ts", h)], w=[("tc", h)])
                c.V(lambda e, h=h: e.match_replace(out=candB[:, h, :], in_to_replace=ts_[:, h, 0:8], in_values=candf[:, h, :], imm_value=-1e30),
                    r=["bufA", ("ts", h)], w=[("cB", h)])
                c.V(lambda e, h=h: e.max(out=ts_[:, h, 8:16], in_=candB[:, h, :]), r=[("cB", h)], w=[("ts2", h)])
                c.V(lambda e, h=h: e.max_index(out=tc_[:, h, 8:16], in_max=ts_[:, h, 8:16], in_values=candB[:, h, :]),
                    r=[("cB", h), ("ts2", h)], w=[("tc2", h)])
            tsall = [("ts", h) for h in range(8)] + [("ts2", h) for h in range(8)]
            tcall = [("tc", h) for h in range(8)] + [("tc2", h) for h in range(8)]
            c.V(lambda e: e.tensor_scalar(out=ai[:], in0=tc_[:], scalar1=4, scalar2=None, op0=ALU.logical_shift_right), r=tcall, w=["ai"])
            c.V(lambda e: e.tensor_scalar(out=bi[:], in0=tc_[:], scalar1=15, scalar2=None, op0=ALU.bitwise_and), r=tcall, w=["bi"])
            c.V(lambda e: e.tensor_copy(out=af[:], in_=ai[:]), r=["ai"], w=["af"])
            c.V(lambda e: e.tensor_copy(out=bf[:], in_=bi[:]), r=["bi"], w=["bf"])
            i16b = iota16.unsqueeze(1).unsqueeze(1).to_broadcast([128, 8, 16, 16])
            for which, (sel, dst) in enumerate(((af, 0), (bf, 1))):
                oh = (bufA if which == 0 else bufB)[:].rearrange("p (h k j) -> p h k j", h=8, k=16)
                okey = "bufA" if which == 0 else "ohB"
                rk = ["af"] if which == 0 else ["bf"]
                c.V(lambda e, sel=sel, oh=oh: e.tensor_tensor(out=oh, in0=sel[:].unsqueeze(3).to_broadcast([128, 8, 16, 16]),
                                                              in1=i16b, op=ALU.is_equal),
                    r=rk + ["iotaF"] + tsall + [("cB", h) for h in range(8)], w=[okey])
                c.V(lambda e, which=which, oh=oh: e.tensor_tensor(out=oh, in0=oh,
                                                                  in1=ixf4[:, :, which, :].unsqueeze(2).to_broadcast([128, 8, 16, 16]),
                                                                  op=ALU.mult), r=[okey, "ixf"], w=[okey])
                c.V(lambda e, dst=dst, oh=oh: e.tensor_reduce(out=IG[:, dst, :].rearrange("p (h k) -> p h k", h=8), in_=oh,
                                                              axis=AX.X, op=ALU.add), r=[okey], w=[("IG", dst)])
            c.V(lambda e: e.tensor_tensor(out=dd[:], in0=ts_[:], in1=ts_[:, :, 0:1].to_broadcast([128, 8, 16]), op=ALU.subtract),
                r=tsall, w=["dd"])
            c.A(lambda e: e.activation(out=ee[:], in_=dd[:], func=AF.Exp), r=["dd"], w=["ee"])
            c.V(lambda e: e.tensor_reduce(out=es[:], in_=ee[:], axis=AX.X, op=ALU.add), r=["ee"], w=["es"])
            c.V(lambda e: e.reciprocal(out=rs[:], in_=es[:]), r=["es"], w=["rs"])
            c.V(lambda e: e.tensor_tensor(out=IG[:, 2, :].rearrange("p (h k) -> p h k", h=8), in0=ee[:],
                                          in1=rs[:].unsqueeze(2).to_broadcast([128, 8, 16]), op=ALU.mult),
                r=["ee", "rs"], w=[("IG", 2)])
            for q in range(3):
                c.P(lambda e, q=q: e.transpose(out=pTf[:, q, :], in_=IG[:, q, :], identity=c.identf[:]),
                    r=[("IG", q), "identf"], w=[("bk", 4)])
            c.A(lambda e, j=j: e.copy(out=IT[:, :, j * 128:(j + 1) * 128], in_=pTf), r=[("bk", 4)], w=["IT"])
        for t in range(TB):
            s = t % NS
            gbk = 4 + (t // 4) % 4
            c.V(lambda e, t=t, s=s: e.tensor_scalar(out=At[s][:], in0=c.iotaF[:], scalar1=IT[:, 0, t:t + 1], scalar2=IT[:, 2, t:t + 1],
                                                    op0=ALU.is_equal, op1=ALU.mult), r=["IT", "iotaF"], w=[("At", s)])
            c.V(lambda e, t=t, s=s: e.tensor_scalar(out=Bt[s][:], in0=c.iotaF[:], scalar1=IT[:, 1, t:t + 1], scalar2=None,
                                                    op0=ALU.is_equal), r=["IT", "iotaF"], w=[("Bt", s)])
            c.P(lambda e, t=t, s=s, gbk=gbk: e.matmul(c.bank(gbk)[:, (t % 4) * 128:(t % 4 + 1) * 128], lhsT=Bt[s][:], rhs=At[s][:],
                                                      start=True, stop=True),
                r=[("At", s), ("Bt", s)], w=[("bk", gbk)])
            if t % 4 == 3:
                t0 = t - 3
                c.A(lambda e, t0=t0, gbk=gbk: e.copy(out=GT[:, :, t0:t0 + 4].rearrange("p i t -> p t i"),
                                                     in_=c.bank(gbk).rearrange("p (t i) -> p t i", t=4)),
                    r=[("bk", gbk)], w=["GT"])
        def HT(i1):
            s = wload(dTb[i1], "sp", [("dTb", i1)])
            hb = 4 + (i1 % 4)
            for k in range(8):
                c.P(lambda e, k=k, s=s, hb=hb: e.matmul(c.bank(hb)[:, 0:TB], lhsT=wbuf[s][:, k * 128:(k + 1) * 128],
                                                        rhs=xnT[:, k, :], start=(k == 0), stop=(k == 7)),
                    r=[("w", s), "xnT"], w=[("bk", hb)])

        for i0 in range(min(LA, n_i1)):
            HT(i0)
        for i1 in range(n_i1):
            p = i1 % 4
            hb = 4 + p
            h_ps = c.bank(hb)[:, 0:TB]
            su = wload(upb[i1 * 128:(i1 + 1) * 128, :], "pool", [("upb", i1)])
            if i1 + LA < n_i1:
                HT(i1 + LA)
            c.A(lambda e, p=p, h_ps=h_ps: e.activation(out=sq[p][:], in_=h_ps, func=AF.Square, scale=0.044715 ** 0.5),
                r=[("bk", hb)], w=[("sq", p)])
            c.V(lambda e, p=p, h_ps=h_ps: e.scalar_tensor_tensor(out=u2[p][:], in0=sq[p][:], scalar=1.0, in1=h_ps, op0=ALU.add, op1=ALU.mult),
                r=[("sq", p), ("bk", hb)], w=[("u2", p)])
            c.A(lambda e, p=p: e.activation(out=sg[p][:], in_=u2[p][:], func=AF.Sigmoid, scale=1.5957691216057308),
                r=[("u2", p)], w=[("sg", p)])
            c.V(lambda e, p=p, h_ps=h_ps, i1=i1: e.tensor_tensor(out=hg[p][:], in0=GT[:, i1, :], in1=h_ps, op=ALU.mult),
                r=["GT", ("bk", hb)], w=[("hg", p)])
            c.V(lambda e, p=p: e.tensor_tensor(out=AT[p][:], in0=hg[p][:], in1=sg[p][:], op=ALU.mult),
                r=[("hg", p), ("sg", p)], w=[("AT", p)])
            for j in range(2):
                for hf in range(2):
                    c.P(lambda e, p=p, j=j, hf=hf, su=su, i1=i1: e.matmul(
                        c.bank(j * 2 + hf), lhsT=AT[p][:, j * 128:(j + 1) * 128], rhs=wbuf[su][:, hf * 512:(hf + 1) * 512],
                        start=(i1 == 0), stop=(i1 == n_i1 - 1)),
                        r=[("AT", p), ("w", su)], w=[("bk", j * 2 + hf)])
        for j in range(2):
            r0 = b * TB + j * 128
            c.V(lambda e, j=j: e.tensor_tensor(out=xo[j][:], in0=x1[j][:], in1=c.bank(j * 2, 2), op=ALU.add),
                r=[("x1", j), ("bk", j * 2), ("bk", j * 2 + 1)], w=[("xo", j)])
            c.store("sp", out[r0:r0 + 128, :], xo[j][:], ("xo", j), ("out", b, j), stream=("out", j))
    return c.finish([("out", b, j) for b in range(NB) for j in range(2)])


def token_inputs(x, mix, w_out, g, w_query, sub_keys, down, up):
    wq = np.ascontiguousarray(w_query.reshape(8, 128, 16, 128).transpose(2, 1, 0, 3)).reshape(16, 128, 1024)
    skT = np.ascontiguousarray(sub_keys.reshape(16, 128, 128).transpose(2, 0, 1)).reshape(128, 2048)
    dT = np.ascontiguousarray(down.reshape(128, 128, 8, 128).transpose(0, 3, 2, 1)).reshape(128, 128, 1024)
    return {"x": np.ascontiguousarray(x), "mix": np.ascontiguousarray(mix), "w_out": np.ascontiguousarray(w_out),
            "gffn": np.ascontiguousarray(g.reshape(1, 1024)), "wq": wq, "skT": skT, "dT": dT,
            "up": np.ascontiguousarray(up)}


def norm_transpose_tile(c, x_src, xt, junk, ssq, rstd, xn, gb, xnT_out, pbank, tag):
    pTb = c.bank(pbank).bitcast(BF16).rearrange("p (k t) -> p k t", k=8)
    c.load("sp", xt[:], x_src, (tag, "xt"))
    c.A(lambda e: e.activation(out=junk[:], in_=xt[:], func=AF.Square, accum_out=ssq[:, 0:1]),
        r=[(tag, "xt")], w=["junk", (tag, "ssq")])
    c.V(lambda e: e.tensor_scalar(out=rstd[:, 0:1], in0=ssq[:, 0:1], scalar1=1.0 / 1024, scalar2=RMS_EPS,
                                  op0=ALU.mult, op1=ALU.add), r=[(tag, "ssq")], w=[(tag, "rstd")])
    c.A(lambda e: e.activation(out=rstd[:, 0:1], in_=rstd[:, 0:1], func=AF.Sqrt), r=[(tag, "rstd")], w=[(tag, "rstd")])
    c.V(lambda e: e.reciprocal(out=rstd[:, 0:1], in_=rstd[:, 0:1]), r=[(tag, "rstd")], w=[(tag, "rstd")])
    c.V(lambda e: e.scalar_tensor_tensor(out=xn[:], in0=xt[:], scalar=rstd[:, 0:1], in1=gb[:], op0=ALU.mult, op1=ALU.mult),
        r=[(tag, "xt"), (tag, "rstd"), "gb"], w=[(tag, "xn")])
    for k in range(8):
        c.P(lambda e, k=k: e.transpose(out=pTb[:, k, :], in_=xn[:, k * 128:(k + 1) * 128], identity=c.identb[:]),
            r=[(tag, "xn"), "identb"], w=[("bk", pbank)])
    c.V(lambda e: e.tensor_copy(out=xnT_out, in_=pTb), r=[("bk", pbank)], w=[(tag, "xnT")])


def gelu_tanh(c, out, in_, n, tmp, rkeys, wkey, tag, extra_mul=None):
    sq, uu, sg = tmp
    c.A(lambda e: e.activation(out=sq, in_=in_, func=AF.Square), r=rkeys, w=[(tag, "sq")])
    c.V(lambda e: e.tensor_scalar(out=uu, in0=sq, scalar1=0.044715, scalar2=1.0, op0=ALU.mult, op1=ALU.add),
        r=[(tag, "sq")], w=[(tag, "uu")])
    c.V(lambda e: e.tensor_tensor(out=uu, in0=uu, in1=in_, op=ALU.mult), r=[(tag, "uu")] + list(rkeys), w=[(tag, "uu")])
    c.A(lambda e: e.activation(out=sg, in_=uu, func=AF.Sigmoid, scale=1.5957691216057308), r=[(tag, "uu")], w=[(tag, "sg")])
    c.V(lambda e: e.tensor_tensor(out=out, in0=sg, in1=in_, op=ALU.mult), r=[(tag, "sg")] + list(rkeys), w=[wkey])


def build_l0_mixer(S_len):
    c = Ctx()
    x_in = c.din("x", [S_len, 1024])
    gmix = c.din("gmix", [1, 1024])
    w6 = c.din("w6", [1024, 768])
    lbl = c.din("lbl", [1, 384])
    gO_d = c.din("gO", [1, 128])
    gV_d = c.din("gV", [1, 128])
    wsT_d = c.din("wsT", [128, 128])
    bs_d = c.din("bs", [128, 1])
    out = c.dout("out", [S_len, 256])
    c.consts()
    NTL = S_len // 128

    gb = c.sb([128, 1024], F32, name="gb")
    c.load("sp", gb[:], gmix.to_broadcast([128, 1024]), "gb")
    w6b = c.sb([128, 8, 768], BF16, name="w6b")
    c.load("pool", w6b[:], w6.rearrange("(k p) n -> p k n", p=128), "w6b")
    gO = c.sb([128, 128], F32, name="gO")
    c.load("sp", gO[:], gO_d.to_broadcast([128, 128]), "gO")
    gV = c.sb([128, 128], F32, name="gV")
    c.load("sp", gV[:], gV_d.to_broadcast([128, 128]), "gV")
    bs = c.sb([128, 1], F32, name="bs")
    c.load("sp", bs[:], bs_d, "bs")
    wsT = c.sb([128, 128], F32, name="wsT")
    c.load("sp", wsT[:], wsT_d, "wsT")
    ll = c.sb([128, 3, 128], F32, name="ll")
    c.load("sp", ll[:].rearrange("p a n -> p (a n)"), lbl.to_broadcast([128, 384]), "ll")

    io = c.sb([128, 128], I32, name="iomask")
    c.G(lambda e: e.iota(io[:], pattern=[[1, 128]], base=0, channel_multiplier=-1), w=["iomask"])
    LT = c.sb([128, 128], F32, name="LT")
    Lblk = c.sb([128, 128], F32, name="Lblk")
    Ust = c.sb([128, 128], F32, name="Ust")
    cind = c.sb([128, 2], F32, name="cind")
    c.V(lambda e: e.tensor_scalar(out=LT[:], in0=io[:], scalar1=0.0, scalar2=None, op0=ALU.is_ge), r=["iomask"], w=["LT"])
    c.V(lambda e: e.tensor_scalar(out=Lblk[:], in0=io[:], scalar1=0.0, scalar2=None, op0=ALU.is_ge), r=["iomask"], w=["Lblk"])
    c.V(lambda e: e.memset(Lblk[0:64, 64:128], 0.0), w=["Lblk"])
    c.V(lambda e: e.tensor_scalar(out=Ust[:], in0=io[:], scalar1=0.0, scalar2=None, op0=ALU.is_lt), r=["iomask"], w=["Ust"])
    c.V(lambda e: e.memset(Ust[64:128, 0:64], 0.0), w=["Ust"])
    c.V(lambda e: e.memset(cind[:], 0.0), w=["cind"])
    c.V(lambda e: e.memset(cind[0:64, 0:1], 1.0), w=["cind"])
    c.V(lambda e: e.memset(cind[64:128, 1:2], 1.0), w=["cind"])
    WcT = c.sb([128, 128], BF16, name="WcT")
    c.V(lambda e: e.tensor_tensor(out=WcT[:], in0=wsT[:], in1=LT[:], op=ALU.mult), r=["wsT", "LT"], w=["WcT"])
    mx = c.sb([128, 128], F32, name="lbmx")
    lb = c.sb([128, 128], F32, name="lb")
    oml = c.sb([128, 128], F32, name="oml")
    c.V(lambda e: e.tensor_tensor(out=mx[:], in0=ll[:, 0, :], in1=ll[:, 1, :], op=ALU.max), r=["ll"], w=["lbmx"])
    c.V(lambda e: e.tensor_tensor(out=mx[:], in0=mx[:], in1=ll[:, 2, :], op=ALU.max), r=["ll", "lbmx"], w=["lbmx"])
    c.V(lambda e: e.tensor_tensor(out=ll[:], in0=ll[:], in1=mx[:].unsqueeze(1).to_broadcast([128, 3, 128]), op=ALU.subtract),
        r=["ll", "lbmx"], w=["ll"])
    c.A(lambda e: e.activation(out=ll[:], in_=ll[:], func=AF.Exp), r=["ll"], w=["ll"])
    c.V(lambda e: e.tensor_tensor(out=mx[:], in0=ll[:, 0, :], in1=ll[:, 1, :], op=ALU.add), r=["ll"], w=["lbmx"])
    c.V(lambda e: e.tensor_tensor(out=mx[:], in0=mx[:], in1=ll[:, 2, :], op=ALU.add), r=["ll", "lbmx"], w=["lbmx"])
    c.V(lambda e: e.reciprocal(out=mx[:], in_=mx[:]), r=["lbmx"], w=["lbmx"])
    c.V(lambda e: e.tensor_tensor(out=lb[:], in0=ll[:, 0, :], in1=mx[:], op=ALU.mult), r=["ll", "lbmx"], w=["lb"])
    c.V(lambda e: e.tensor_scalar(out=oml[:], in0=lb[:], scalar1=-1.0, scalar2=1.0, op0=ALU.mult, op1=ALU.add), r=["lb"], w=["oml"])

    St = c.sb([128, 128], F32, name="St")
    Sb = [c.sb([128, 128], BF16, name="Sb%d" % i) for i in range(2)]
    c.V(lambda e: e.memset(St[:], 0.0), w=["St"])
    c.V(lambda e: e.memset(Sb[0][:], 0.0), w=[("Sb", 0)])
    qeT0 = [c.sb([128, 128], BF16, name="qeT0_%d" % i) for i in range(2)]
    qeT1 = [c.sb([128, 128], BF16, name="qeT1_%d" % i) for i in range(2)]
    for i in range(2):
        c.V(lambda e, i=i: e.memset(qeT0[i][:], 0.0), w=[("qeT0", i)])
        c.V(lambda e, i=i: e.memset(qeT1[i][:], 0.0), w=[("qeT1", i)])

    def dbl(shape, dt, name):
        return [c.sb(shape, dt, name="%s_%d" % (name, i)) for i in range(2)]

    xt = dbl([128, 1024], F32, "xt")
    junk = c.sb([128, 1024], F32, name="junk")
    ssq = dbl([128, 1], F32, "ssq")
    rstd = dbl([128, 1], F32, "rstd")
    xn = dbl([128, 1024], BF16, "xn")
    xnT = dbl([128, 8, 128], BF16, "xnT")
    sig = dbl([128, 128], F32, "sig")
    ff = dbl([128, 128], F32, "ff")
    gg = dbl([128, 128], F32, "gg")
    kk = dbl([128, 128], F32, "kk")
    vs = dbl([128, 128], BF16, "vs")
    eb = dbl([128, 128], F32, "eb")
    qe = dbl([128, 128], BF16, "qe")
    ke = dbl([128, 128], BF16, "ke")
    kd = dbl([128, 128], BF16, "kd")
    dec = dbl([128, 2], F32, "dec")
    qeTf = dbl([128, 128], BF16, "qeTf")
    keT = dbl([128, 128], BF16, "keT")
    scm = dbl([128, 128], BF16, "scm")
    so = dbl([128, 128], F32, "so")
    osq = dbl([128, 1], F32, "osq")
    t1 = dbl([128, 128], F32, "t1")
    otile = dbl([128, 256], F32, "otile")
    gl = dbl([128, 256], F32, "gl")
    gt0 = dbl([128, 256], F32, "gt0")
    gt1 = dbl([128, 256], F32, "gt1")
    gt2 = dbl([128, 256], F32, "gt2")
    vsq = dbl([128, 1], F32, "vsq")
    vn = dbl([128, 128], BF16, "vn")
    junk2 = c.sb([128, 128], F32, name="junk2")

    for ti in range(NTL):
        p = ti % 2
        T = ("t", p)
        norm_transpose_tile(c, x_in[ti * 128:(ti + 1) * 128, :], xt[p], junk, ssq[p], rstd[p], xn[p], gb, xnT[p][:], 2, T)
        for k in range(8):
            c.P(lambda e, k=k, p=p: e.matmul(c.bank(0), lhsT=xnT[p][:, k, :], rhs=w6b[:, k, 0:512], start=(k == 0), stop=(k == 7)),
                r=[(T, "xnT"), "w6b"], w=[("bk", 0)])
        for k in range(8):
            c.P(lambda e, k=k, p=p: e.matmul(c.bank(1)[:, 0:256], lhsT=xnT[p][:, k, :], rhs=w6b[:, k, 512:768], start=(k == 0), stop=(k == 7)),
                r=[(T, "xnT"), "w6b"], w=[("bk", 1)])
        z0 = c.bank(0)
        z1 = c.bank(1)
        zq, zfg, zin, zog = z0[:, 0:128], z0[:, 128:256], z0[:, 256:384], z0[:, 384:512]
        c.A(lambda e, p=p: e.activation(out=sig[p][:], in_=zfg, func=AF.Sigmoid), r=[("bk", 0)], w=[(T, "sig")])
        c.V(lambda e, p=p: e.tensor_tensor(out=ff[p][:], in0=sig[p][:], in1=oml[:], op=ALU.mult), r=[(T, "sig"), "oml"], w=[(T, "ff")])
        c.V(lambda e, p=p: e.tensor_tensor(out=ff[p][:], in0=ff[p][:], in1=lb[:], op=ALU.add), r=[(T, "ff"), "lb"], w=[(T, "ff")])
        c.A(lambda e, p=p: e.activation(out=gg[p][:], in_=ff[p][:], func=AF.Ln), r=[(T, "ff")], w=[(T, "gg")])
        c.V(lambda e, p=p: e.tensor_scalar(out=kk[p][:], in0=ff[p][:], scalar1=-1.0, scalar2=1.0, op0=ALU.mult, op1=ALU.add),
            r=[(T, "ff")], w=[(T, "kk")])
        c.A(lambda e, p=p: e.activation(out=vs[p][:], in_=zin, func=AF.Silu), r=[("bk", 0)], w=[(T, "vs")])
        c.A(lambda e, p=p: e.activation(out=so[p][:], in_=zog, func=AF.Silu), r=[("bk", 0)], w=[(T, "so")])
        bk3 = c.bank(3)
        c.P(lambda e, p=p: e.matmul(bk3[:, 0:128], lhsT=Lblk[:], rhs=gg[p][:], start=True, stop=True), r=["Lblk", (T, "gg")], w=[("bk", 3)])
        c.P(lambda e, p=p: e.matmul(bk3[:, 128:256], lhsT=Ust[:], rhs=gg[p][:], start=True, stop=True), r=["Ust", (T, "gg")], w=[("bk", 3)])
        c.P(lambda e, p=p: e.matmul(bk3[:, 256:258], lhsT=gg[p][:], rhs=cind[:], start=True, stop=True), r=["cind", (T, "gg")], w=[("bk", 3)])
        c.A(lambda e, p=p: e.activation(out=eb[p][:], in_=bk3[:, 0:128], func=AF.Exp), r=[("bk", 3)], w=[(T, "eb")])
        c.V(lambda e, p=p: e.tensor_tensor(out=qe[p][:], in0=eb[p][:], in1=zq, op=ALU.mult), r=[(T, "eb"), ("bk", 0)], w=[(T, "qe")])
        c.A(lambda e, p=p: e.activation(out=eb[p][:], in_=bk3[:, 0:128], func=AF.Exp, scale=-1.0), r=[("bk", 3), (T, "qe")], w=[(T, "eb")])
        c.V(lambda e, p=p: e.tensor_tensor(out=ke[p][:], in0=eb[p][:], in1=kk[p][:], op=ALU.mult), r=[(T, "eb"), (T, "kk")], w=[(T, "ke")])
        c.A(lambda e, p=p: e.activation(out=eb[p][:], in_=bk3[:, 128:256], func=AF.Exp), r=[("bk", 3), (T, "ke")], w=[(T, "eb")])
        c.V(lambda e, p=p: e.tensor_tensor(out=kd[p][:], in0=eb[p][:], in1=kk[p][:], op=ALU.mult), r=[(T, "eb"), (T, "kk")], w=[(T, "kd")])
        c.A(lambda e, p=p: e.activation(out=dec[p][:], in_=bk3[:, 256:258], func=AF.Exp), r=[("bk", 3)], w=[(T, "dec")])
        pq = c.bank(4).bitcast(BF16)
        c.P(lambda e, p=p: e.transpose(out=pq[:, 0:128], in_=qe[p][:], identity=c.identb[:]), r=[(T, "qe"), "identb"], w=[("bk", 4)])
        c.P(lambda e, p=p: e.transpose(out=pq[:, 128:256], in_=ke[p][:], identity=c.identb[:]), r=[(T, "ke"), "identb"], w=[("bk", 4)])
        c.V(lambda e, p=p: e.tensor_copy(out=qeTf[p][:], in_=pq[:, 0:128]), r=[("bk", 4)], w=[(T, "qeTf")])
        c.A(lambda e, p=p: e.copy(out=qeT0[p][:, 0:64], in_=pq[:, 0:64]), r=[("bk", 4)], w=[("qeT0", p)])
        c.A(lambda e, p=p: e.copy(out=qeT1[p][:, 64:128], in_=pq[:, 64:128]), r=[("bk", 4)], w=[("qeT1", p)])
        c.V(lambda e, p=p: e.tensor_copy(out=keT[p][:], in_=pq[:, 128:256]), r=[("bk", 4)], w=[(T, "keT")])
        c.P(lambda e, p=p: e.matmul(c.bank(5)[:, 0:128], lhsT=keT[p][:], rhs=qeTf[p][:], start=True, stop=True),
            r=[(T, "keT"), (T, "qeTf")], w=[("bk", 5)])
        c.V(lambda e, p=p: e.tensor_tensor(out=scm[p][:], in0=Lblk[:], in1=c.bank(5)[:, 0:128], op=ALU.mult),
            r=["Lblk", ("bk", 5)], w=[(T, "scm")])
        c.P(lambda e, p=p: e.matmul(c.bank(7)[:, 0:128], lhsT=kd[p][0:64, :], rhs=vs[p][0:64, :], start=True, stop=True),
            r=[(T, "kd"), (T, "vs")], w=[("bk", 7)])
        c.V(lambda e, p=p: e.scalar_tensor_tensor(out=St[:], in0=St[:], scalar=dec[p][:, 0:1], in1=c.bank(7)[:, 0:128],
                                                  op0=ALU.mult, op1=ALU.add), r=["St", (T, "dec"), ("bk", 7)], w=["St"])
        c.A(lambda e: e.copy(out=Sb[1][:], in_=St[:]), r=["St"], w=[("Sb", 1)])
        c.P(lambda e, p=p: e.matmul(c.bank(6)[:, 0:128], lhsT=scm[p][:], rhs=vs[p][:], start=True, stop=False),
            r=[(T, "scm"), (T, "vs")], w=[("bk", 6)])
        c.P(lambda e, p=p: e.matmul(c.bank(6)[:, 0:128], lhsT=qeT0[p][:], rhs=Sb[0][:], start=False, stop=False),
            r=[("qeT0", p), ("Sb", 0)], w=[("bk", 6)])
        c.P(lambda e, p=p: e.matmul(c.bank(6)[:, 0:128], lhsT=qeT1[p][:], rhs=Sb[1][:], start=False, stop=True),
            r=[("qeT1", p), ("Sb", 1)], w=[("bk", 6)])
        c.P(lambda e, p=p: e.matmul(c.bank(5)[:, 128:256], lhsT=kd[p][64:128, :], rhs=vs[p][64:128, :], start=True, stop=True),
            r=[(T, "kd"), (T, "vs")], w=[("bk", 5)])
        c.V(lambda e, p=p: e.scalar_tensor_tensor(out=St[:], in0=St[:], scalar=dec[p][:, 1:2], in1=c.bank(5)[:, 128:256],
                                                  op0=ALU.mult, op1=ALU.add), r=["St", (T, "dec"), ("bk", 5)], w=["St"])
        c.A(lambda e: e.copy(out=Sb[0][:], in_=St[:]), r=["St"], w=[("Sb", 0)])
        o_ps = c.bank(6)[:, 0:128]
        c.A(lambda e, p=p: e.activation(out=junk2[:], in_=o_ps, func=AF.Square, accum_out=osq[p][:]), r=[("bk", 6)], w=["junk2", (T, "osq")])
        c.V(lambda e, p=p: e.tensor_scalar(out=osq[p][:], in0=osq[p][:], scalar1=1.0 / 128, scalar2=RMS_EPS, op0=ALU.mult, op1=ALU.add),
            r=[(T, "osq")], w=[(T, "osq")])
        c.A(lambda e, p=p: e.activation(out=osq[p][:], in_=osq[p][:], func=AF.Sqrt), r=[(T, "osq")], w=[(T, "osq")])
        c.V(lambda e, p=p: e.reciprocal(out=osq[p][:], in_=osq[p][:]), r=[(T, "osq")], w=[(T, "osq")])
        c.V(lambda e, p=p: e.scalar_tensor_tensor(out=t1[p][:], in0=o_ps, scalar=osq[p][:, 0:1], in1=gO[:], op0=ALU.mult, op1=ALU.mult),
            r=[("bk", 6), (T, "osq"), "gO"], w=[(T, "t1")])
        c.V(lambda e, p=p: e.tensor_tensor(out=otile[p][:, 0:128], in0=t1[p][:], in1=so[p][:], op=ALU.mult),
            r=[(T, "t1"), (T, "so")], w=[(T, "oa")])
        gelu_tanh(c, gl[p][:], z1[:, 0:256], 256, (gt0[p][:], gt1[p][:], gt2[p][:]), [("bk", 1)], (T, "gl"), (T, "g"))
        c.A(lambda e, p=p: e.activation(out=junk2[:], in_=gl[p][:, 128:256], func=AF.Square, accum_out=vsq[p][:]),
            r=[(T, "gl")], w=["junk2", (T, "vsq")])
        c.V(lambda e, p=p: e.tensor_scalar(out=vsq[p][:], in0=vsq[p][:], scalar1=1.0 / 128, scalar2=RMS_EPS, op0=ALU.mult, op1=ALU.add),
            r=[(T, "vsq")], w=[(T, "vsq")])
        c.A(lambda e, p=p: e.activation(out=vsq[p][:], in_=vsq[p][:], func=AF.Sqrt), r=[(T, "vsq")], w=[(T, "vsq")])
        c.V(lambda e, p=p: e.reciprocal(out=vsq[p][:], in_=vsq[p][:]), r=[(T, "vsq")], w=[(T, "vsq")])
        c.V(lambda e, p=p: e.scalar_tensor_tensor(out=vn[p][:], in0=gl[p][:, 128:256], scalar=vsq[p][:, 0:1], in1=gV[:],
                                                  op0=ALU.mult, op1=ALU.mult), r=[(T, "gl"), (T, "vsq"), "gV"], w=[(T, "vn")])
        c.P(lambda e, p=p: e.matmul(c.bank(7)[:, 256:384], lhsT=WcT[:], rhs=vn[p][:], start=True, stop=True),
            r=["WcT", (T, "vn")], w=[("bk", 7)])
        c.V(lambda e, p=p: e.scalar_tensor_tensor(out=otile[p][:, 128:256], in0=c.bank(7)[:, 256:384], scalar=bs[:, 0:1],
                                                  in1=gl[p][:, 0:128], op0=ALU.add, op1=ALU.mult),
            r=[("bk", 7), "bs", (T, "gl")], w=[(T, "ob")])
        c.S.dma("sp", lambda e, s, p=p, ti=ti: e.dma_start(out=out[ti * 128:(ti + 1) * 128, :], in_=otile[p][:]).then_inc(s, 16),
                reads=[(T, "oa"), (T, "ob")], writes=[("out", ti)], stream=("out", p))
    return c.finish([("out", ti) for ti in range(NTL)])


def l0_inputs(xb, norm_mix, w_in, lb_logits, hgrn_out_norm, gmlp_v_norm, gmlp_w_s, gmlp_b_s, h):
    cols = np.concatenate([np.arange(h * 128, (h + 1) * 128) + off for off in (0, 512, 1024, 1536, 2048, 2560)])
    return {"x": np.ascontiguousarray(xb), "gmix": np.ascontiguousarray(norm_mix.reshape(1, 1024)),
            "w6": np.ascontiguousarray(w_in[:, cols]),
            "lbl": np.ascontiguousarray(lb_logits[:, h * 128:(h + 1) * 128]).reshape(1, 384),
            "gO": np.ascontiguousarray(hgrn_out_norm[h * 128:(h + 1) * 128]).reshape(1, 128),
            "gV": np.ascontiguousarray(gmlp_v_norm[h * 128:(h + 1) * 128]).reshape(1, 128),
            "wsT": np.ascontiguousarray(gmlp_w_s[h].T), "bs": np.ascontiguousarray(gmlp_b_s[h]).reshape(128, 1)}


def rms_rows(c, src, n, gain, out, junk, st, rk, tag, wkey):
    c.A(lambda e: e.activation(out=junk, in_=src, func=AF.Square, accum_out=st), r=rk, w=["junk", (tag, "st")])
    c.V(lambda e: e.tensor_scalar(out=st, in0=st, scalar1=1.0 / n, scalar2=RMS_EPS, op0=ALU.mult, op1=ALU.add),
        r=[(tag, "st")], w=[(tag, "st")])
    c.A(lambda e: e.activation(out=st, in_=st, func=AF.Sqrt), r=[(tag, "st")], w=[(tag, "st")])
    c.V(lambda e: e.reciprocal(out=st, in_=st), r=[(tag, "st")], w=[(tag, "st")])
    c.V(lambda e: e.scalar_tensor_tensor(out=out, in0=src, scalar=st, in1=gain, op0=ALU.mult, op1=ALU.mult),
        r=list(rk) + [(tag, "st"), "gains"], w=[wkey])


SUB = [9]


def build_l1_mixer(S_len, stage=9):
    c = Ctx()
    x_in = c.din("x", [S_len, 1024])
    gmix = c.din("gmix", [1, 1024])
    w832 = c.din("w832", [1024, 832])
    gains_d = c.din("gains", [1, 1024])
    wuq_d = c.din("wuq", [256, 192])
    wukv_d = c.din("wukv", [128, 256])
    pos_d = c.din("pos", [128, S_len // 128], I32)
    invf_d = c.din("invf", [1, 32])
    out = c.dout("out", [S_len, 256])
    c.consts()
    NTL = S_len // 128
    NQB = S_len // 512
    NBLK = S_len // 256
    BIG = 1e30

    gb = c.sb([128, 1024], F32, name="gb")
    c.load("sp", gb[:], gmix.to_broadcast([128, 1024]), "gb")
    gains = c.sb([128, 1024], F32, name="gains")
    c.load("sp", gains[:], gains_d.to_broadcast([128, 1024]), "gains")
    g_cq, g_ckv, g_q, g_k, g_mq, g_mk = (gains[:, 0:256], gains[:, 256:384], gains[:, 384:576], gains[:, 576:768],
                                         gains[:, 768:896], gains[:, 896:1024])
    wb = c.sb([128, 8, 832], BF16, name="wb")
    c.load("pool", wb[:], w832.rearrange("(k p) n -> p k n", p=128), "wb")
    wuq = c.sb([128, 2, 192], BF16, name="wuq")
    c.load("pool", wuq[:], wuq_d.rearrange("(k p) n -> p k n", p=128), "wuq")
    wukv = c.sb([128, 256], BF16, name="wukv")
    c.load("pool", wukv[:], wukv_d, "wukv")
    posi = c.sb([128, NTL], I32, name="posi")
    c.load("sp", posi[:], pos_d, "posi")
    invf = c.sb([128, 32], F32, name="invf")
    c.load("sp", invf[:], invf_d.to_broadcast([128, 32]), "invf")
    posf = c.sb([128, NTL], F32, name="posf")
    c.V(lambda e: e.tensor_copy(out=posf[:], in_=posi[:]), r=["posi"], w=["posf"])
    sinT = c.sb([128, NTL, 32], F32, name="sinT")
    cosT = c.sb([128, NTL, 32], F32, name="cosT")
    rtmp = c.sb([128, NTL, 32], F32, name="rtmp")
    ni = c.sb([128, NTL, 32], I32, name="ni")
    TWO_PI = 6.283185307179586
    C1 = 6.28125
    C2 = TWO_PI - C1
    c.V(lambda e: e.tensor_tensor(out=rtmp[:], in0=posf[:].unsqueeze(2).to_broadcast([128, NTL, 32]),
                                  in1=invf[:].unsqueeze(1).to_broadcast([128, NTL, 32]), op=ALU.mult), r=["posf", "invf"], w=["ang"])
    for tab, shift, key in ((sinT, 0.0, "sinT"), (cosT, 0.5 * np.pi, "cosT")):
        c.V(lambda e, tab=tab, shift=shift: e.tensor_scalar(out=tab[:], in0=rtmp[:], scalar1=float(shift), scalar2=1.0 / TWO_PI,
                                                            op0=ALU.add, op1=ALU.mult), r=["ang"], w=[key])
        c.V(lambda e, tab=tab: e.tensor_copy(out=ni[:], in_=tab[:]), r=[key], w=["ni"])
        c.V(lambda e, tab=tab: e.tensor_copy(out=tab[:], in_=ni[:]), r=["ni"], w=[key])
        c.V(lambda e, tab=tab: e.scalar_tensor_tensor(out=ni[:].bitcast(F32), in0=tab[:], scalar=-C1, in1=rtmp[:], op0=ALU.mult, op1=ALU.add),
            r=[key, "ang"], w=["ni"])
        c.V(lambda e, tab=tab: e.scalar_tensor_tensor(out=tab[:], in0=tab[:], scalar=-C2, in1=ni[:].bitcast(F32), op0=ALU.mult, op1=ALU.add),
            r=[key, "ni"], w=[key])
        c.V(lambda e, tab=tab, shift=shift: e.tensor_scalar(out=tab[:], in0=tab[:], scalar1=float(shift), scalar2=None, op0=ALU.add),
            r=[key], w=[key])
        c.V(lambda e, tab=tab: e.tensor_scalar(out=ni[:].bitcast(F32), in0=tab[:], scalar1=float(np.pi), scalar2=-TWO_PI, op0=ALU.is_gt, op1=ALU.mult),
            r=[key], w=["ni"])
        c.V(lambda e, tab=tab: e.tensor_tensor(out=tab[:], in0=tab[:], in1=ni[:].bitcast(F32), op=ALU.add), r=[key, "ni"], w=[key])
        c.V(lambda e, tab=tab: e.tensor_scalar(out=ni[:].bitcast(F32), in0=tab[:], scalar1=-float(np.pi), scalar2=TWO_PI, op0=ALU.is_lt, op1=ALU.mult),
            r=[key], w=["ni"])
        c.V(lambda e, tab=tab: e.tensor_tensor(out=tab[:], in0=tab[:], in1=ni[:].bitcast(F32), op=ALU.add), r=[key, "ni"], w=[key])
        c.V(lambda e, tab=tab: e.tensor_scalar(out=tab[:], in0=tab[:], scalar1=3.1415925, scalar2=-3.1415925, op0=ALU.min, op1=ALU.max),
            r=[key], w=[key])
        c.A(lambda e, tab=tab: e.activation(out=tab[:], in_=tab[:], func=AF.Sin), r=[key], w=[key])
    io = c.sb([128, 512], I32, name="iom")
    maskD = c.sb([128, 4, 512], BF16, name="maskD")
    for m in range(4):
        c.G(lambda e, m=m: e.iota(io[:], pattern=[[1, 512]], base=-128 * m, channel_multiplier=-1), w=["iom"])
        c.V(lambda e, m=m: e.tensor_scalar(out=maskD[:, m, :], in0=io[:], scalar1=0.0, scalar2=None, op0=ALU.is_ge), r=["iom"], w=["maskD"])
    ioe = c.sb([128, 32], I32, name="ioe")
    Eblk = c.sb([128, 32, 128], BF16, name="Eblk")
    c.G(lambda e: e.iota(ioe[:], pattern=[[-1, 32]], base=0, channel_multiplier=1), w=["ioe"])
    c.V(lambda e: e.tensor_scalar(out=Eblk[:], in0=ioe[:].unsqueeze(2).to_broadcast([128, 32, 128]), scalar1=0.0, scalar2=None,
                                  op0=ALU.is_equal), r=["ioe"], w=["Eblk"])
    ones256 = c.sb([128, 1], F32, name="ones256")
    c.V(lambda e: e.memset(ones256[:], 1.0 / 256), w=["ones256"])
    kcTa = c.sb([128, S_len], BF16, name="kcTa")
    kcTb = c.sb([128, S_len], BF16, name="kcTb")
    kdT = c.sb([128, S_len], BF16, name="kdT")
    Vc = c.sb([128, NTL, 129], BF16, name="Vc")
    Vd = c.sb([128, NTL, 129], BF16, name="Vd")
    c.G(lambda e: e.memset(Vc[:, :, 128:129], 1.0), w=["Vc"])
    c.G(lambda e: e.memset(Vd[:, :, 128:129], 1.0), w=["Vd"])
    kmT = c.sb([128, 32], F32, name="kmT")
    c.V(lambda e: e.memset(kmT[:], 0.0), w=["kmT"])
    qa = c.sb([128, 512], BF16, name="qa")
    qb = c.sb([128, 512], BF16, name="qb")
    qd = c.sb([128, 512], BF16, name="qd")
    MT = c.sb([128, 512], BF16, name="MT")
    xt = c.sb([128, 1024], F32, name="xt")
    junk = c.sb([128, 1024], F32, name="junk")
    ssq = c.sb([128, 1], F32, name="ssq")
    rstd = c.sb([128, 1], F32, name="rstd")
    xn = c.sb([128, 1024], BF16, name="xn")
    xnT = c.sb([128, 8, 128], BF16, name="xnT")
    st = c.sb([128, 8], F32, name="st")
    cqn = c.sb([128, 256], BF16, name="cqn")
    cqT = c.sb([128, 2, 128], BF16, name="cqT")
    ckvn = c.sb([128, 128], BF16, name="ckvn")
    ckvT = c.sb([128, 128], BF16, name="ckvT")
    qk = c.sb([128, 2, 192], F32, name="qk")
    kcat = c.sb([128, 192], F32, name="kcat")
    qkb = c.sb([128, 2, 256], BF16, name="qkb")
    c.V(lambda e: e.memset(qkb[:], 0.0), w=["qkb0", "qkb1", "qkb2"])
    rt = c.sb([128, 4, 2, 32], F32, name="rt")
    qdn = c.sb([128, 128], F32, name="qdn")
    qdb = c.sb([128, 128], BF16, name="qdb")
    kdn = c.sb([128, 128], F32, name="kdn")
    kdb = c.sb([128, 128], BF16, name="kdb")
    qdTf = c.sb([128, 128], F32, name="qdTf")
    gsb = c.sb([128, 32], F32, name="gsb")
    m8 = c.sb([128, 8], F32, name="m8")
    self_ = c.sb([128, 32], F32, name="self")
    Mb = c.sb([128, 128], BF16, name="Mb")
    c.V(lambda e: e.memset(Mb[:], 0.0), w=["Mb"])
    PT = [c.sb([128, 512], BF16, name="PT%d" % i) for i in range(4)]
    rec = c.sb([128, 1], F32, name="rec")
    ktmp = c.sb([128, 1], F32, name="ktmp")
    otile = [c.sb([128, 256], F32, name="otile%d" % i) for i in range(4)]
    pTb = c.bank(2).bitcast(BF16)

    def proj_tile(ti, jj):
        T = "p"
        norm_transpose_tile(c, x_in[ti * 128:(ti + 1) * 128, :], xt, junk, ssq, rstd, xn, gb, xnT[:], 2, T)
        for k in range(8):
            c.P(lambda e, k=k: e.matmul(c.bank(0), lhsT=xnT[:, k, :], rhs=wb[:, k, 0:512], start=(k == 0), stop=(k == 7)),
                r=[(T, "xnT"), "wb"], w=[("bk", 0)])
        for k in range(8):
            c.P(lambda e, k=k: e.matmul(c.bank(1)[:, 0:320], lhsT=xnT[:, k, :], rhs=wb[:, k, 512:832], start=(k == 0), stop=(k == 7)),
                r=[(T, "xnT"), "wb"], w=[("bk", 1)])
        z0, z1 = c.bank(0), c.bank(1)
        if stage == 1 and SUB[0] <= 1:
            return z0, z1
        rms_rows(c, z0[:, 0:256], 256, g_cq, cqn[:], junk[:, 0:256], st[:, 0:1], [("bk", 0)], "cq", "cqn")
        for kc in range(2):
            c.P(lambda e, kc=kc: e.transpose(out=pTb[:, kc * 128:(kc + 1) * 128], in_=cqn[:, kc * 128:(kc + 1) * 128], identity=c.identb[:]),
                r=["cqn", "identb"], w=[("bk", 2)])
        c.V(lambda e: e.tensor_copy(out=cqT[:].rearrange("p a t -> p (a t)"), in_=pTb[:, 0:256]), r=[("bk", 2)], w=["cqT"])
        for kc in range(2):
            c.P(lambda e, kc=kc: e.matmul(c.bank(3)[:, 0:192], lhsT=cqT[:, kc, :], rhs=wuq[:, kc, :], start=(kc == 0), stop=(kc == 1)),
                r=["cqT", "wuq"], w=[("bk", 3)])
        rms_rows(c, c.bank(3)[:, 0:192], 192, g_q, qk[:, 0, :], junk[:, 0:192], st[:, 1:2], [("bk", 3)], "qc", ("qk", 0))
        if stage == 1 and SUB[0] <= 2:
            return z0, z1
        rms_rows(c, z0[:, 256:384], 128, g_ckv, ckvn[:], junk[:, 0:128], st[:, 2:3], [("bk", 0)], "ckv", "ckvn")
        c.P(lambda e: e.transpose(out=pTb[:, 256:384], in_=ckvn[:], identity=c.identb[:]), r=["ckvn", "identb"], w=[("bk", 2)])
        c.V(lambda e: e.tensor_copy(out=ckvT[:], in_=pTb[:, 256:384]), r=[("bk", 2)], w=["ckvT"])
        c.P(lambda e: e.matmul(c.bank(3)[:, 256:512], lhsT=ckvT[:], rhs=wukv[:], start=True, stop=True), r=["ckvT", "wukv"], w=[("bk", 3)])
        c.A(lambda e: e.copy(out=kcat[:, 0:128], in_=c.bank(3)[:, 256:384]), r=[("bk", 3)], w=["kcat"])
        c.A(lambda e: e.copy(out=kcat[:, 128:192], in_=z0[:, 384:448]), r=[("bk", 0)], w=["kcat"])
        c.A(lambda e: e.copy(out=Vc[:, ti, 0:128], in_=c.bank(3)[:, 384:512]), r=[("bk", 3)], w=["Vc"])
        rms_rows(c, kcat[:], 192, g_k, qk[:, 1, :], junk[:, 0:192], st[:, 3:4], ["kcat"], "kc", ("qk", 1))
        if stage == 1 and SUB[0] <= 3:
            return z0, z1
        x1 = qk[:, :, 128:160]
        x2 = qk[:, :, 160:192]
        cs = cosT[:, ti, :].unsqueeze(1).to_broadcast([128, 2, 32])
        sn = sinT[:, ti, :].unsqueeze(1).to_broadcast([128, 2, 32])
        rk = [("qk", 0), ("qk", 1), "sinT", "cosT"]
        c.V(lambda e: e.tensor_tensor(out=rt[:, 0], in0=x1, in1=cs, op=ALU.mult), r=rk, w=["rt0"])
        c.V(lambda e: e.tensor_tensor(out=rt[:, 1], in0=x2, in1=sn, op=ALU.mult), r=rk, w=["rt1"])
        c.V(lambda e: e.tensor_tensor(out=rt[:, 2], in0=x2, in1=cs, op=ALU.mult), r=rk, w=["rt2"])
        c.V(lambda e: e.tensor_tensor(out=rt[:, 3], in0=x1, in1=sn, op=ALU.mult), r=rk, w=["rt3"])
        c.V(lambda e: e.tensor_copy(out=qkb[:, :, 0:128], in_=qk[:, :, 0:128]), r=[("qk", 0), ("qk", 1)], w=["qkb0"])
        c.V(lambda e: e.tensor_tensor(out=qkb[:, :, 128:160], in0=rt[:, 0], in1=rt[:, 1], op=ALU.subtract), r=["rt0", "rt1"], w=["qkb1"])
        c.V(lambda e: e.tensor_tensor(out=qkb[:, :, 160:192], in0=rt[:, 2], in1=rt[:, 3], op=ALU.add), r=["rt2", "rt3"], w=["qkb2"])
        if stage == 1 and SUB[0] <= 4:
            return z0, z1
        qkk = ["qkb0", "qkb1", "qkb2", "identb"]
        c.P(lambda e: e.transpose(out=pTb[:, 0:128], in_=qkb[:, 0, 0:128], identity=c.identb[:]), r=qkk, w=[("bk", 2)])
        c.P(lambda e: e.transpose(out=pTb[:, 128:256], in_=qkb[:, 0, 128:256], identity=c.identb[:]), r=qkk, w=[("bk", 2)])
        c.P(lambda e: e.transpose(out=pTb[:, 256:384], in_=qkb[:, 1, 0:128], identity=c.identb[:]), r=qkk, w=[("bk", 2)])
        c.P(lambda e: e.transpose(out=pTb[:, 384:512], in_=qkb[:, 1, 128:256], identity=c.identb[:]), r=qkk, w=[("bk", 2)])
        sl = slice(jj * 128, (jj + 1) * 128)
        gs = slice(ti * 128, (ti + 1) * 128)
        if stage == 1 and SUB[0] <= 5:
            return z0, z1
        c.V(lambda e: e.tensor_copy(out=qa[:, sl], in_=pTb[:, 0:128]), r=[("bk", 2)], w=["qa"])
        if stage == 1 and SUB[0] <= 6:
            return z0, z1
        c.V(lambda e: e.tensor_copy(out=qb[:, sl], in_=pTb[:, 128:256]), r=[("bk", 2)], w=["qb"])
        if stage == 1 and SUB[0] <= 7:
            return z0, z1
        c.V(lambda e: e.tensor_copy(out=kcTa[:, gs], in_=pTb[:, 256:384]), r=[("bk", 2)], w=["kcTa"])
        c.V(lambda e: e.tensor_copy(out=kcTb[:, gs], in_=pTb[:, 384:512]), r=[("bk", 2)], w=["kcTb"])
        return z0, z1

    def moba_tile(ti, jj, z0, z1):
        sl = slice(jj * 128, (jj + 1) * 128)
        gs = slice(ti * 128, (ti + 1) * 128)
        jb = ti // 2
        c.A(lambda e: e.copy(out=qdn[:, 0:64], in_=z0[:, 448:512]), r=[("bk", 0)], w=["qdn"])
        c.A(lambda e: e.copy(out=qdn[:, 64:128], in_=z1[:, 0:64]), r=[("bk", 1)], w=["qdn"])
        rms_rows(c, qdn[:], 128, g_mq, qdn[:], junk[:, 0:128], st[:, 4:5], ["qdn"], "mq", "qdn")
        rms_rows(c, z1[:, 64:192], 128, g_mk, kdn[:], junk[:, 0:128], st[:, 5:6], [("bk", 1)], "mk", "kdn")
        c.A(lambda e: e.copy(out=Vd[:, ti, 0:128], in_=z1[:, 192:320]), r=[("bk", 1)], w=["Vd"])
        c.V(lambda e: e.tensor_copy(out=qdb[:], in_=qdn[:]), r=["qdn"], w=["qdb"])
        c.V(lambda e: e.tensor_copy(out=kdb[:], in_=kdn[:]), r=["kdn"], w=["kdb"])
        c.P(lambda e: e.transpose(out=pTb[:, 512:640], in_=qdb[:], identity=c.identb[:]), r=["qdb", "identb"], w=[("bk", 2)])
        c.P(lambda e: e.transpose(out=pTb[:, 640:768], in_=kdb[:], identity=c.identb[:]), r=["kdb", "identb"], w=[("bk", 2)])
        c.V(lambda e: e.tensor_copy(out=qd[:, sl], in_=pTb[:, 512:640]), r=[("bk", 2)], w=["qd"])
        c.V(lambda e: e.tensor_copy(out=kdT[:, gs], in_=pTb[:, 640:768]), r=[("bk", 2)], w=["kdT"])
        c.P(lambda e: e.transpose(out=c.bank(3)[:, 0:128], in_=qdn[:], identity=c.identf[:]), r=["qdn", "identf"], w=[("bk", 3)])
        c.V(lambda e: e.tensor_copy(out=qdTf[:], in_=c.bank(3)[:, 0:128]), r=[("bk", 3)], w=["qdTf"])
        c.P(lambda e: e.matmul(c.bank(3)[:, 128:160], lhsT=qdTf[:], rhs=kmT[:], start=True, stop=True), r=["qdTf", "kmT"], w=[("bk", 3)])
        c.V(lambda e: e.tensor_copy(out=gsb[:], in_=c.bank(3)[:, 128:160]), r=[("bk", 3)], w=["gsb"])
        c.V(lambda e: e.memset(gsb[:, jb:32], -BIG), r=["gsb"], w=["gsb"])
        c.V(lambda e: e.max(out=m8[:], in_=gsb[:]), r=["gsb"], w=["m8"])
        c.V(lambda e: e.memset(self_[:], 0.0), w=["self"])
        if jb > 0:
            c.V(lambda e: e.tensor_scalar(out=self_[:, 0:jb], in0=gsb[:, 0:jb], scalar1=m8[:, 2:3], scalar2=None, op0=ALU.is_ge),
                r=["gsb", "m8", "self"], w=["self"])
        c.V(lambda e: e.memset(self_[:, jb:jb + 1], 1.0), r=["self"], w=["self"])
        c.V(lambda e: e.tensor_scalar(out=Mb[:, 0:32], in0=self_[:], scalar1=-1.0, scalar2=BIG, op0=ALU.add, op1=ALU.mult), r=["self", "Mb"], w=["Mb"])
        c.P(lambda e: e.transpose(out=pTb[:, 768:896], in_=Mb[:], identity=c.identb[:]), r=["Mb", "identb"], w=[("bk", 2)])
        c.V(lambda e: e.tensor_copy(out=MT[:, sl], in_=pTb[:, 768:896]), r=[("bk", 2)], w=["MT"])
        half = ti % 2
        c.P(lambda e: e.matmul(c.bank(3)[:, 192:193], lhsT=kdn[:], rhs=ones256[:], start=True, stop=True),
            r=["kdn", "ones256"], w=[("bk", 3)])
        if half == 0:
            c.V(lambda e: e.tensor_copy(out=ktmp[:], in_=c.bank(3)[:, 192:193]), r=[("bk", 3)], w=["ktmp"])
        else:
            c.V(lambda e: e.tensor_tensor(out=kmT[:, jb:jb + 1], in0=ktmp[:], in1=c.bank(3)[:, 192:193], op=ALU.add),
                r=[("bk", 3), "ktmp"], w=["kmT"])

    def attention(j, which):
        scale = (192 ** -0.5) if which == 0 else (128 ** -0.5)
        nk = 4 * j + 4
        Vt = Vc if which == 0 else Vd
        vk = "Vc" if which == 0 else "Vd"

        def score(i):
            sb_ = i % 4
            ks = slice(i * 128, (i + 1) * 128)
            if which == 0:
                c.P(lambda e: e.matmul(c.bank(sb_), lhsT=kcTa[:, ks], rhs=qa[:], start=True, stop=False),
                    r=["kcTa", "qa"], w=[("bk", sb_)])
                c.P(lambda e: e.matmul(c.bank(sb_), lhsT=kcTb[:, ks], rhs=qb[:], start=False, stop=True),
                    r=["kcTb", "qb"], w=[("bk", sb_)])
            else:
                c.P(lambda e: e.matmul(c.bank(sb_), lhsT=kdT[:, ks], rhs=qd[:], start=True, stop=False),
                    r=["kdT", "qd"], w=[("bk", sb_)])
                c.P(lambda e: e.matmul(c.bank(sb_), lhsT=Eblk[:, i // 2, :], rhs=MT[:], start=False, stop=True),
                    r=["Eblk", "MT"], w=[("bk", sb_)])

        for i0 in range(min(3, nk)):
            score(i0)
        for i in range(nk):
            sb_ = i % 4
            pp = i % 4
            if i + 3 < nk:
                score(i + 3)
            c.A(lambda e, pp=pp, sb_=sb_: e.activation(out=PT[pp][:], in_=c.bank(sb_), func=AF.Exp, scale=float(scale)),
                r=[("bk", sb_)], w=[("PT", pp)])
            m = i - 4 * j
            if m >= 0:
                c.V(lambda e, pp=pp, m=m: e.tensor_tensor(out=PT[pp][:], in0=PT[pp][:], in1=maskD[:, m, :], op=ALU.mult),
                    r=[("PT", pp), "maskD"], w=[("PT", pp)])
            for jj in range(4):
                if i > 4 * j + jj:
                    continue
                c.P(lambda e, pp=pp, jj=jj, i=i: e.matmul(c.bank(4 + jj)[:, 0:129], lhsT=PT[pp][:, jj * 128:(jj + 1) * 128], rhs=Vt[:, i, :],
                                                          start=(i == 0), stop=(i == 4 * j + jj)),
                    r=[("PT", pp), vk], w=[("bk", 4 + jj)])
        for jj in range(4):
            acc = c.bank(4 + jj)
            c.V(lambda e, acc=acc: e.reciprocal(out=rec[:], in_=acc[:, 128:129]), r=[("bk", 4 + jj)], w=["rec"])
            c.V(lambda e, acc=acc, jj=jj: e.tensor_scalar(out=otile[jj][:, which * 128:(which + 1) * 128], in0=acc[:, 0:128],
                                                          scalar1=rec[:, 0:1], scalar2=None, op0=ALU.mult),
                r=[("bk", 4 + jj), "rec"], w=[("ot", jj, which)])

    if stage < 9:
        for jj in range(4):
            c.V(lambda e, jj=jj: e.memset(otile[jj][:], 0.0), w=[("ot", jj, 0), ("ot", jj, 1)])
    for j in range(NQB):
        for jj in range(4):
            ti = 4 * j + jj
            if stage >= 1:
                z0, z1 = proj_tile(ti, jj)
            if stage >= 2:
                moba_tile(ti, jj, z0, z1)
        if stage >= 3:
            attention(j, 0)
        if stage >= 4:
            attention(j, 1)
        for jj in range(4):
            ti = 4 * j + jj
            c.S.dma("sp", lambda e, s, jj=jj, ti=ti: e.dma_start(out=out[ti * 128:(ti + 1) * 128, :], in_=otile[jj][:]).then_inc(s, 16),
                    reads=[("ot", jj, 0), ("ot", jj, 1)], writes=[("out", ti)], stream=("out", jj))
    return c.finish([("out", ti) for ti in range(NTL)])


def l1_inputs(xb, pos_b, norm_mix, w_in, cq_norm, ckv_norm, w_uq, w_ukv, mla_q_norm, mla_k_norm, moba_q_norm, moba_k_norm, h):
    cols = np.concatenate([np.arange(0, 448), 448 + h * 128 + np.arange(128), 960 + h * 128 + np.arange(128),
                           1472 + h * 128 + np.arange(128)])
    gains = np.concatenate([cq_norm, ckv_norm, mla_q_norm, mla_k_norm, moba_q_norm, moba_k_norm]).reshape(1, 1024)
    invf = (10000.0 ** (-np.arange(32, dtype=np.float32) / 32)).astype(np.float32).reshape(1, 32)
    return {"x": np.ascontiguousarray(xb), "gmix": np.ascontiguousarray(norm_mix.reshape(1, 1024)),
            "w832": np.ascontiguousarray(w_in[:, cols]), "gains": np.ascontiguousarray(gains.astype(np.float32)),
            "wuq": np.ascontiguousarray(w_uq[:, h * 192:(h + 1) * 192]),
            "wukv": np.ascontiguousarray(w_ukv[:, h * 256:(h + 1) * 256]),
            "pos": np.ascontiguousarray(pos_b.astype(np.int32).reshape(-1, 128).T), "invf": invf}


_PROGS = {}


def _prog(name, fn, *a):
    k = (name,) + a
    if k not in _PROGS:
        _PROGS[k] = fn(*a)
    return _PROGS[k]


def _run(nc, maps):
    return run_bass_kernel_spmd(nc, maps, core_ids=list(range(len(maps)))).results


def kernel(**inp):
    inp = {k: np.asarray(v) for k, v in inp.items()}
    x = inp["x"].astype(np.float32)
    B, S, D = x.shape
    NTOK = B * S
    NC = 8
    per = NTOK // NC

    def assemble(res):
        mix = np.empty((B, S, D), np.float32)
        for b in range(B):
            for h in range(4):
                o = res[b * 4 + h]["out"]
                mix[b, :, h * 128:(h + 1) * 128] = o[:, :128]
                mix[b, :, 512 + h * 128:512 + (h + 1) * 128] = o[:, 128:]
        return mix

    def token_phase(xcur, mix, L):
        nct = _prog("tok", build_token_phase, per)
        xf = xcur.reshape(NTOK, D)
        mf = mix.reshape(NTOK, D)
        base = token_inputs(xf[0:per], mf[0:per], inp[L + "_w_out"], inp[L + "_norm_ffn"], inp[L + "_peer_w_query"],
                            inp[L + "_peer_sub_keys"], inp[L + "_peer_expert_down"], inp[L + "_peer_expert_up"])
        maps = []
        for ci in range(NC):
            m = dict(base)
            m["x"] = np.ascontiguousarray(xf[ci * per:(ci + 1) * per])
            m["mix"] = np.ascontiguousarray(mf[ci * per:(ci + 1) * per])
            maps.append(m)
        res = _run(nct, maps)
        return np.concatenate([r["out"] for r in res], axis=0).reshape(B, S, D)

    nc0 = _prog("l0", build_l0_mixer, S)
    maps = [l0_inputs(x[b], inp["l0_norm_mix"], inp["l0_w_in"], inp["lb_logits"], inp["l0_hgrn_out_norm"], inp["l0_gmlp_v_norm"],
                      inp["l0_gmlp_w_s"], inp["l0_gmlp_b_s"], h) for b in range(B) for h in range(4)]
    mix = assemble(_run(nc0, maps))
    x = token_phase(x, mix, "l0")
    nc1 = _prog("l1", build_l1_mixer, S)
    maps = [l1_inputs(x[b], inp["positions"][b], inp["l1_norm_mix"], inp["l1_w_in"], inp["l1_mla_cq_norm"], inp["l1_mla_ckv_norm"],
                      inp["l1_mla_w_uq"], inp["l1_mla_w_ukv"], inp["l1_mla_q_norm"], inp["l1_mla_k_norm"], inp["l1_moba_q_norm"],
                      inp["l1_moba_k_norm"], h) for b in range(B) for h in range(4)]
    mix = assemble(_run(nc1, maps))
    x = token_phase(x, mix, "l1")
    return x.astype(np.float32)
```
